# Optimizing a Trainium2 kernel written in Bass

```python
import jax, jax.numpy as jnp
from jax import lax
import numpy as np

D_MODEL = 2048
BATCH = 4
SEQ = 4096
DEPTH = 2

N_MIXERS = 2
N_HEADS = 16
HEAD_DIM = D_MODEL // N_HEADS
DILATED_GROUPS = ((128, 1), (512, 4), (2048, 16))
N_ATT_GROUPS = len(DILATED_GROUPS)
ATT_BLOCK = 128
ROPE_THETA = 10000.0
POOL_WINDOWS = (2, 4, 8, 16)
N_POOL_GROUPS = len(POOL_WINDOWS)
POOL_DIM = D_MODEL // N_POOL_GROUPS
D_FF = 256 * ((8 * D_MODEL // 3 + 255) // 256)
N_EXPERTS = 8
TOP_K = 2
PLE_DIM = 256
DN_ALPHA = (2 * DEPTH) ** 0.25
DN_BETA = (8 * DEPTH) ** -0.25
LN_EPS = 1e-5
NEG_INF = -1e30
N_EVEN = (DEPTH + 1) // 2
N_ODD = DEPTH // 2

kernel_name = "dilated_attn_pool_moe_hybrid"


def layer_norm(x, g, b):
    xf = x.astype(jnp.float32)
    mu = jnp.mean(xf, axis=-1, keepdims=True)
    xc = xf - mu
    var = jnp.mean(xc * xc, axis=-1, keepdims=True)
    return (xc * lax.rsqrt(var + LN_EPS) * g + b).astype(x.dtype)


def rope(t, pos):
    half = HEAD_DIM // 2
    inv = ROPE_THETA ** (-jnp.arange(half, dtype=jnp.float32) / half)
    ang = pos.astype(jnp.float32)[:, None] * inv[None, :]
    cos = jnp.cos(ang)[None, :, None, :].astype(t.dtype)
    sin = jnp.sin(ang)[None, :, None, :].astype(t.dtype)
    t1, t2 = t[..., :half], t[..., half:]
    return jnp.concatenate([t1 * cos - t2 * sin, t2 * cos + t1 * sin], axis=-1)


def dilated_window_attention(q, k, v, window, dilation):
    B, S, H, Dh = q.shape
    span = dilation * ATT_BLOCK
    Sp = -(-S // span) * span
    pad = Sp - S
    n_sub = Sp // dilation
    nb = n_sub // ATT_BLOCK
    n_back = window // dilation

    def to_blocks(t):
        t = jnp.pad(t, ((0, 0), (0, pad), (0, 0), (0, 0)))
        t = t.reshape(B, n_sub, dilation, H, Dh).transpose(0, 2, 1, 3, 4)
        return t.reshape(B, dilation, nb, ATT_BLOCK, H, Dh)

    def with_prev(t):
        prev = jnp.pad(t, ((0, 0), (0, 0), (1, 0), (0, 0), (0, 0), (0, 0)))[:, :, :-1]
        return jnp.concatenate([prev, t], axis=3)

    qb = to_blocks(q)
    kw = with_prev(to_blocks(k))
    vw = with_prev(to_blocks(v))
    scores = jnp.einsum('brnqhd,brnkhd->brnhqk', qb, kw).astype(jnp.float32) * (Dh ** -0.5)
    qi = jnp.arange(ATT_BLOCK)[:, None] + ATT_BLOCK
    ki = jnp.arange(2 * ATT_BLOCK)[None, :]
    dist = qi - ki
    band = (dist >= 0) & (dist <= n_back)
    has_prev = (jnp.arange(nb)[:, None, None] > 0) | (ki[None] >= ATT_BLOCK)
    mask = band[None] & has_prev
    scores = jnp.where(mask[None, None, :, None], scores, NEG_INF)
    m = jnp.max(scores, axis=-1, keepdims=True)
    e = jnp.exp(scores - m)
    l = jnp.sum(e, axis=-1, keepdims=True)
    o = jnp.einsum('brnhqk,brnkhd->brnqhd', (e / l).astype(v.dtype), vw)
    lse = (m + jnp.log(l))[..., 0]
    o = o.reshape(B, dilation, n_sub, H, Dh).transpose(0, 2, 1, 3, 4).reshape(B, Sp, H, Dh)[:, :S]
    lse = lse.transpose(0, 1, 2, 4, 3).reshape(B, dilation, n_sub, H)
    lse = lse.transpose(0, 2, 1, 3).reshape(B, Sp, H)[:, :S]
    return o.astype(jnp.float32), lse


def dilated_attention_mixer(x, w_qkv, w_o):
    B, S, _ = x.shape
    qkv = (x @ w_qkv).reshape(B, S, N_ATT_GROUPS, 3, N_HEADS, HEAD_DIM)
    pos = jnp.arange(S)
    outs, lses = [], []
    for g, (win, dil) in enumerate(DILATED_GROUPS):
        q = rope(qkv[:, :, g, 0], pos)
        k = rope(qkv[:, :, g, 1], pos)
        o, lse = dilated_window_attention(q, k, qkv[:, :, g, 2], win, dil)
        outs.append(o)
        lses.append(lse)
    wts = jax.nn.softmax(jnp.stack(lses, axis=0), axis=0)
    o = jnp.einsum('gbsh,gbshd->bshd', wts, jnp.stack(outs, axis=0)).astype(x.dtype)
    return o.reshape(B, S, N_HEADS * HEAD_DIM) @ w_o


def multiscale_pool_mixer(x, w_in, w_group, scale, w_o):
    B, S, _ = x.shape
    u = (x @ w_in).reshape(B, S, N_POOL_GROUPS, POOL_DIM)
    c = jnp.cumsum(u.astype(jnp.float32), axis=1)
    t = jnp.arange(S)
    pooled = []
    for g, w in enumerate(POOL_WINDOWS):
        cg = jnp.pad(c[:, :, g], ((0, 0), (w, 0), (0, 0)))
        s = cg[:, w:] - cg[:, :S]
        cnt = jnp.minimum(t + 1, w).astype(jnp.float32)
        pooled.append(s / cnt[None, :, None])
    pooled = jnp.stack(pooled, axis=2)
    mixed = (pooled - u.astype(jnp.float32)).astype(x.dtype)
    y = jnp.einsum('bsgc,gcd->bsgd', mixed, w_group) * scale
    return y.reshape(B, S, D_MODEL) @ w_o


def swiglu(x, w1, w3, w2):
    return (jax.nn.silu(x @ w1) * (x @ w3)) @ w2


def moe_swiglu(x, w_router, w1, w3, w2):
    logits = (x @ w_router).astype(jnp.float32)
    top_v, top_i = lax.top_k(logits, TOP_K)
    gates = jax.nn.softmax(top_v, axis=-1)
    dense_gate = jnp.sum(jax.nn.one_hot(top_i, N_EXPERTS, dtype=jnp.float32) * gates[..., None], axis=-2)
    dense_gate = dense_gate.astype(x.dtype)
    y = jnp.zeros_like(x)
    for e in range(N_EXPERTS):
        y = y + dense_gate[..., e:e + 1] * swiglu(x, w1[e], w3[e], w2[e])
    return y


def setup_inputs(seed: int = 0) -> dict:
    key = jax.random.key(seed)
    ks = jax.random.split(key, 24)
    f32 = jnp.float32
    nrm = lambda k, shape, s: jax.random.normal(k, shape, f32) * s
    att_w = N_HEADS * HEAD_DIM
    return {
        "x": nrm(ks[0], (BATCH, SEQ, D_MODEL), 1.0),
        "p": nrm(ks[1], (DEPTH, BATCH, SEQ, PLE_DIM), 1.0),
        "attn_w_qkv": nrm(ks[2], (N_EVEN, D_MODEL, N_ATT_GROUPS * 3 * att_w), D_MODEL ** -0.5),
        "attn_w_o": nrm(ks[3], (N_EVEN, att_w, D_MODEL), DN_BETA * att_w ** -0.5),
        "pool_w_in": nrm(ks[4], (N_ODD, D_MODEL, D_MODEL), D_MODEL ** -0.5),
        "pool_w_group": nrm(ks[5], (N_ODD, N_POOL_GROUPS, POOL_DIM, POOL_DIM), POOL_DIM ** -0.5),
        "pool_scale": 1.0 + nrm(ks[6], (N_ODD, N_POOL_GROUPS, POOL_DIM), 0.02),
        "pool_w_o": nrm(ks[7], (N_ODD, D_MODEL, D_MODEL), DN_BETA * D_MODEL ** -0.5),
        "ln_mix_g": 1.0 + nrm(ks[8], (DEPTH, D_MODEL), 0.02),
        "ln_mix_b": nrm(ks[9], (DEPTH, D_MODEL), 0.02),
        "ln_ffn_g": 1.0 + nrm(ks[10], (DEPTH, D_MODEL), 0.02),
        "ln_ffn_b": nrm(ks[11], (DEPTH, D_MODEL), 0.02),
        "ffn_w1": nrm(ks[12], (N_EVEN, D_MODEL, D_FF), D_MODEL ** -0.5),
        "ffn_w3": nrm(ks[13], (N_EVEN, D_MODEL, D_FF), D_MODEL ** -0.5),
        "ffn_w2": nrm(ks[14], (N_EVEN, D_FF, D_MODEL), DN_BETA * D_FF ** -0.5),
        "moe_router": nrm(ks[15], (N_ODD, D_MODEL, N_EXPERTS), D_MODEL ** -0.5),
        "moe_w1": nrm(ks[16], (N_ODD, N_EXPERTS, D_MODEL, D_FF), D_MODEL ** -0.5),
        "moe_w3": nrm(ks[17], (N_ODD, N_EXPERTS, D_MODEL, D_FF), D_MODEL ** -0.5),
        "moe_w2": nrm(ks[18], (N_ODD, N_EXPERTS, D_FF, D_MODEL), DN_BETA * D_FF ** -0.5),
        "ple_w_proj": nrm(ks[19], (DEPTH, PLE_DIM, D_MODEL), PLE_DIM ** -0.5),
        "ple_w_gate": nrm(ks[20], (DEPTH, D_MODEL, D_MODEL), D_MODEL ** -0.5),
        "ple_b_gate": nrm(ks[21], (DEPTH, D_MODEL), 0.01),
    }


def reference(x, p, attn_w_qkv, attn_w_o, pool_w_in, pool_w_group, pool_scale, pool_w_o,
              ln_mix_g, ln_mix_b, ln_ffn_g, ln_ffn_b, ffn_w1, ffn_w3, ffn_w2,
              moe_router, moe_w1, moe_w3, moe_w2, ple_w_proj, ple_w_gate, ple_b_gate):
    for i in range(DEPTH):
        j = i // N_MIXERS
        if i % N_MIXERS == 0:
            h = dilated_attention_mixer(x, attn_w_qkv[j], attn_w_o[j])
        else:
            h = multiscale_pool_mixer(x, pool_w_in[j], pool_w_group[j], pool_scale[j], pool_w_o[j])
        x = layer_norm(DN_ALPHA * x + h, ln_mix_g[i], ln_mix_b[i])
        if i % 2 == 0:
            f = swiglu(x, ffn_w1[j], ffn_w3[j], ffn_w2[j])
        else:
            f = moe_swiglu(x, moe_router[j], moe_w1[j], moe_w3[j], moe_w2[j])
        x = layer_norm(DN_ALPHA * x + f, ln_ffn_g[i], ln_ffn_b[i])
        gate = jax.nn.sigmoid((x @ ple_w_gate[i]).astype(jnp.float32) + ple_b_gate[i]).astype(x.dtype)
        x = x + gate * (p[i] @ ple_w_proj[i])
    return x
```

```python
import contextlib
import numpy as np
import concourse.bass as bass
import concourse.mybir as mybir
from concourse.bass_utils import run_bass_kernel_spmd

F32 = mybir.dt.float32
BF16 = mybir.dt.bfloat16
AF = mybir.ActivationFunctionType
ALU = mybir.AluOpType

S = 4096
OWN = 2048
HALO0 = 1920
NQ = S - HALO0
D = 2048
H = 16
DH = 128
DFF = 5632
NE = 8
ST = 1024
NST = S // ST
KC = D // 128
FC = DFF // 128
NSPLIT = 4
HF = FC // NSPLIT
ALPHA = float((2 * 2) ** 0.25)
EPS = 1e-5
DILS = (1, 4, 16)
POOLW = (2, 4, 8, 16)
ENGS = ("pe", "act", "dve", "pool", "sp")

V_LN_MIX_G0, V_LN_MIX_B0, V_LN_FFN_G0, V_LN_FFN_B0 = 0, 1, 2, 3
V_LN_MIX_G1, V_LN_MIX_B1, V_LN_FFN_G1, V_LN_FFN_B1 = 4, 5, 6, 7
V_PLE_B0, V_PLE_B1, V_POOL_SCALE = 8, 9, 10
NV = 11


def I(name, **kw):
    return (name, kw)


class Buf:
    def __init__(self, ap=None, excl=False):
        self.ap = ap
        self.wr = {}
        self.rd = {}
        self.excl = excl


class Prog:
    def __init__(self, nc):
        self.nc = nc
        self.q = {e: [] for e in ENGS}
        self.sems = {}
        self.cnt = {}
        self.stack = contextlib.ExitStack()
        self.dma_rr = {"pool": 0, "sp": 0}
        self.ndma_sems = {"pool": 12, "sp": 24}
        self.waited = {e: {} for e in ENGS}
        self.ninstr = 0

    def sem(self, name):
        if name not in self.sems:
            self.sems[name] = self.stack.enter_context(self.nc.semaphore(name))
            self.cnt[name] = 0
        return name

    def _deps(self, rd, wr, eng_sem=None, acc=False):
        deps = {}

        def add(d, skip=None):
            for s, v in d.items():
                if s == skip:
                    continue
                if deps.get(s, 0) < v:
                    deps[s] = v

        for b in rd:
            add(b.wr)
            if b.excl:
                add(b.rd, eng_sem)
        for b in wr:
            add(b.wr, eng_sem if acc else None)
            add(b.rd)
        return deps

    def _commit(self, tok, rd, wr, acc=False):
        s, v = tok
        for b in rd:
            if b.rd.get(s, 0) < v:
                b.rd[s] = v
        for b in wr:
            if acc:
                b.wr[s] = v
            else:
                b.wr = {s: v}
                b.rd = {}

    def op(self, eng, ins, rd=(), wr=(), acc=False):
        if isinstance(ins, tuple):
            ins = [ins]
        s = self.sem("p_" + eng)
        deps = self._deps(rd, wr, s, acc)
        self.cnt[s] += 1
        tok = (s, self.cnt[s])
        self.q[eng].append((ins, deps, tok, 1))
        self._commit(tok, rd, wr, acc)
        self.ninstr += len(ins)
        return tok

    def dma(self, eng, out, in_, rd=(), wr=()):
        i = self.dma_rr[eng]
        self.dma_rr[eng] = (i + 1) % self.ndma_sems[eng]
        s = self.sem("d_%s_%d" % (eng, i))
        deps = self._deps(rd, wr)
        if self.cnt[s] > 0:
            deps[s] = max(deps.get(s, 0), self.cnt[s])
        self.cnt[s] += 16
        tok = (s, self.cnt[s])
        self.q[eng].append(([I("dma_start", out=out, in_=in_)], deps, tok, 16))
        self._commit(tok, rd, wr)
        return tok

    def barrier(self):
        allv = {s: v for s, v in self.cnt.items() if v > 0}
        for e in ENGS:
            self.q[e].append(([], dict(allv), None, 0))

    def flush(self):
        nc = self.nc
        if not any(self.q[e] for e in ENGS):
            return
        with nc.Block() as block:
            decos = {"pe": block.tensor, "act": block.scalar, "dve": block.vector,
                     "pool": block.gpsimd, "sp": block.sync}
            for e in ENGS:
                items = self.q[e]
                if not items:
                    continue

                def body(engine, e=e, items=items):
                    waited = self.waited[e]
                    for inss, deps, tok, amt in items:
                        for s, v in deps.items():
                            if waited.get(s, 0) < v:
                                engine.wait_ge(self.sems[s], v)
                                waited[s] = v
                        last = None
                        for name, kw in inss:
                            last = getattr(engine, name)(**kw)
                        if tok is not None:
                            last.then_inc(self.sems[tok[0]], amt)

                decos[e](body)
        self.q = {e: [] for e in ENGS}


class RR:
    def __init__(self, aps, excl=False):
        self.bufs = [a if isinstance(a, Buf) else Buf(a, excl) for a in aps]
        self.i = 0

    def next(self):
        b = self.bufs[self.i]
        self.i = (self.i + 1) % len(self.bufs)
        return b


class Stream:
    def __init__(self, P, slots, reqs, depth=1):
        self.P = P
        self.slots = slots
        self.reqs = reqs
        self.issued = 0
        self.got = {}
        self.depth = depth

    def _issue_upto(self, last):
        while self.issued <= min(last, len(self.reqs) - 1):
            src, view = self.reqs[self.issued]
            b = self.slots.next()
            self.P.dma("pool", view(b.ap), src, rd=(), wr=(b,))
            self.got[self.issued] = b
            self.issued += 1

    def prefetch(self, n):
        self._issue_upto(n - 1)

    def get(self, i):
        self._issue_upto(i + self.depth)
        return self.got.pop(i)


def build(ncores=4, test=None):
    test = test or {}
    phases = test.get("phases", ("qkv", "attn", "l0", "l1"))
    ne_run = test.get("ne", NE)
    nc = bass.Bass("TRN2", target_bir_lowering=False)
    P = Prog(nc)

    def din(name, shape, dt=F32):
        if name in test.get("skip", ()):
            return nc.dram_tensor(name, [128, 128], dt)
        return nc.dram_tensor(name, list(shape), dt, kind="ExternalInput")

    def dscratch(name, shape, dt):
        role = test.get("roles", {}).get(name)
        if role == "in":
            return nc.dram_tensor(name, list(shape), dt, kind="ExternalInput")
        if role == "out":
            return nc.dram_tensor(name, list(shape), dt, kind="ExternalOutput")
        return nc.dram_tensor(name, list(shape), dt)

    xT = din("xT", [D, S])
    pT = din("pT", [2, 256, NQ])
    w_qkv = din("attn_w_qkv", [D, 3 * 3 * D])
    w_o = din("attn_w_o", [D, D])
    pool_w_in = din("pool_w_in", [D, D])
    pool_w_group = din("pool_w_group", [4, 512, 512])
    pool_w_o = din("pool_w_o", [D, D])
    ffn_w1 = din("ffn_w1", [D, DFF])
    ffn_w3 = din("ffn_w3", [D, DFF])
    ffn_w2 = din("ffn_w2", [DFF, D])
    moe_router = din("moe_router", [D, NE])
    moe_w1 = din("moe_w1", [NE, D, DFF])
    moe_w3 = din("moe_w3", [NE, D, DFF])
    moe_w2 = din("moe_w2", [NE, DFF, D])
    ple_w_proj = din("ple_w_proj", [2, 256, D])
    ple_w_gate = din("ple_w_gate", [2, D, D])
    vecs_d = din("vecs", [128, NV, KC])
    cos_d = din("cosT", [128, S])
    sin_d = din("sinT", [128, S])
    rmat_d = din("rmat", [128, 128])
    mask_d = din("maskT", [128, 2, 128])
    maskp_d = din("maskP", [128, 2, 128])
    hflag_d = din("hflag", [128, 16])
    ident_d = din("ident", [128, 128])
    selb_d = din("selb", [8, NE, 128])
    corr_d = din("corr", [128, 4, 16])
    outT = nc.dram_tensor("outT", [D, OWN], F32, kind="ExternalOutput")

    QT = [dscratch("QT%d" % g, [D, S], BF16) for g in range(3)]
    KT = [dscratch("KT%d" % g, [D, S], BF16) for g in range(3)]
    VT = [dscratch("VT%d" % g, [D, S], BF16) for g in range(3)]
    oT = dscratch("oT", [D, S], BF16)
    x3T = dscratch("x3T", [D, S], F32)
    dbg = {}
    for nm in test.get("dbg", ()):
        dbg[nm] = nc.dram_tensor("dbg_" + nm, [D, S], F32, kind="ExternalOutput")

    dbufs = {}

    def dbuf(key):
        if key not in dbufs:
            dbufs[key] = Buf()
        return dbufs[key]

    def chunkview(ap2d):
        return ap2d.rearrange("(c p) n -> p c n", p=128)

    def tt2(ap):
        return ap.rearrange("p (a b) -> p a b", b=512)

    def tt3(ap):
        return ap.rearrange("p c (a b) -> p c a b", b=512)

    es0 = contextlib.ExitStack()

    def sb(es, name, shape, dt):
        return es.enter_context(nc.sbuf_tensor(name, list(shape), dt))

    vecs = sb(es0, "vecs_sb", [128, NV, KC], F32)
    ones32 = sb(es0, "ones32", [128, 128], F32)
    onesb = sb(es0, "onesb", [128, 128], BF16)
    identb = sb(es0, "identb", [128, 128], BF16)
    rmat = sb(es0, "rmat_sb", [128, 128], BF16)
    biasb = sb(es0, "biasb", [128, 2, 128], BF16)
    biasp = sb(es0, "biasp", [128, 2, 128], BF16)
    hflag = sb(es0, "hflag_sb", [128, 16], F32)
    selb = sb(es0, "selb_sb", [8, NE, 128], F32)
    corr = sb(es0, "corr_sb", [128, 4, 16], F32)
    wr32 = sb(es0, "wr32", [128, KC, NE], F32)
    B_vecs, B_selb, B_corr, B_wr, B_id, B_rm, B_mask, B_o32, B_ob = (Buf() for _ in range(9))
    P.dma("sp", vecs[:], vecs_d.ap(), wr=(B_vecs,))
    P.dma("sp", selb[:], selb_d.ap(), wr=(B_selb,))
    P.dma("sp", corr[:], corr_d.ap(), wr=(B_corr,))
    P.dma("sp", wr32[:], chunkview(moe_router.ap()), wr=(B_wr,))
    P.dma("pool", identb[:], ident_d.ap(), wr=(B_id,))
    P.dma("pool", rmat[:], rmat_d.ap(), wr=(B_rm,))
    P.dma("pool", biasb[:], mask_d.ap(), wr=(B_mask,))
    B_maskp, B_hflag = Buf(), Buf()
    P.dma("pool", biasp[:], maskp_d.ap(), wr=(B_maskp,))
    P.dma("sp", hflag[:], hflag_d.ap(), wr=(B_hflag,))
    P.op("dve", I("memset", ap=ones32[:], constant=1.0), wr=(B_o32,))
    P.op("dve", I("memset", ap=onesb[:], constant=1.0), wr=(B_ob,))
    P.barrier()
    P.flush()

    ps = es0.enter_context(nc.psum_tensor("ps", [128, 8, 512], F32))

    def mm(out, lhsT, rhs, start, stop):
        return I("matmul", out=out, lhsT=lhsT, rhs=rhs, start=start, stop=stop)

    if "qkv" in phases:
        with contextlib.ExitStack() as es:
            xb = sb(es, "xb1", [128, KC, 2, 512], BF16)
            cos = sb(es, "cos", [128, 2 * NST, 512], F32)
            sin = sb(es, "sin", [128, 2 * NST, 512], F32)
            wsl = RR([sb(es, "wq%d" % i, [128, KC, 256], BF16) for i in range(2)])
            qbp = RR([sb(es, "qb%d" % i, [128, 2, 512], BF16) for i in range(2)])
            t1p = RR([sb(es, "t1_%d" % i, [128, 2, 512], F32) for i in range(2)])
            t2p = RR([sb(es, "t2_%d" % i, [128, 2, 512], F32) for i in range(2)])
            obp = RR([sb(es, "ob%d" % i, [128, 2, 512], BF16) for i in range(3)])
            psA = RR([ps[:, 0:2, :], ps[:, 4:6, :]], excl=True)
            psB = RR([ps[:, 2:4, :], ps[:, 6:8, :]], excl=True)
            B_cos, B_sin, B_xb = Buf(), Buf(), Buf()
            P.dma("sp", cos[:], cos_d.ap().rearrange("p (a b) -> p a b", b=512), wr=(B_cos,))
            P.dma("sp", sin[:], sin_d.ap().rearrange("p (a b) -> p a b", b=512), wr=(B_sin,))
            wqv = chunkview(w_qkv.ap())
            xTv = chunkview(xT.ap())
            WTS = test.get("wts", list(range(72)))
            for st in range(NST):
                P.dma("pool", xb[:], tt3(xTv[:, :, st * ST:(st + 1) * ST]), wr=(B_xb,))
                WT_ST = [wt for wt in WTS if st > 0 or (wt // 24 == 2 and (wt % 24) // 8 >= 1)]
                reqs = [(wqv[:, :, wt * 256:(wt + 1) * 256], (lambda ap: ap[:])) for wt in WT_ST]
                stream = Stream(P, wsl, reqs)
                pending = None
                for wi, wt in enumerate(WT_ST):
                    g = wt // 24
                    typ = (wt % 24) // 8
                    hp = wt % 8
                    wb = stream.get(wi)
                    for hh in range(2):
                        h = hp * 2 + hh
                        A = psA.next()
                        P.op("pe", [mm(A.ap[:, t, :], wb.ap[:, kc, hh * 128:(hh + 1) * 128], xb[:, kc, t, :],
                                       kc == 0, kc == KC - 1) for t in range(2) for kc in range(KC)],
                             rd=(wb, B_xb), wr=(A,))
                        if pending is not None:
                            pending()
                            pending = None
                        ob = obp.next()
                        dst = (QT, KT, VT)[typ][g]
                        dst_ap = tt2(dst[h * 128:(h + 1) * 128, st * ST:(st + 1) * ST])
                        key = ("qkv", typ, g, h, st)
                        if typ < 2:
                            qb = qbp.next()
                            P.op("act", I("activation", out=qb.ap[:], in_=A.ap, func=AF.Copy), rd=(A,), wr=(qb,))

                            def cont(A=A, qb=qb, ob=ob, dst_ap=dst_ap, key=key, st=st):
                                Bp = psB.next()
                                P.op("pe", [mm(Bp.ap[:, t, :], rmat[:], qb.ap[:, t, :], True, True) for t in range(2)],
                                     rd=(qb, B_rm), wr=(Bp,))
                                t1 = t1p.next()
                                t2 = t2p.next()
                                P.op("dve", I("tensor_tensor", out=t1.ap[:], in0=A.ap, in1=cos[:, 2 * st:2 * st + 2, :],
                                              op=ALU.mult), rd=(A, B_cos), wr=(t1,))
                                P.op("dve", I("tensor_tensor", out=t2.ap[:], in0=Bp.ap, in1=sin[:, 2 * st:2 * st + 2, :],
                                              op=ALU.mult), rd=(Bp, B_sin), wr=(t2,))
                                P.op("pool", I("tensor_tensor", out=ob.ap[:], in0=t1.ap[:], in1=t2.ap[:], op=ALU.add),
                                     rd=(t1, t2), wr=(ob,))
                                P.dma("sp", dst_ap, ob.ap[:], rd=(ob,), wr=(dbuf(key),))
                            pending = cont
                        else:
                            P.op("act", I("activation", out=ob.ap[:], in_=A.ap, func=AF.Copy), rd=(A,), wr=(ob,))
                            P.dma("sp", dst_ap, ob.ap[:], rd=(ob,), wr=(dbuf(key),))
                if pending is not None:
                    pending()
                    pending = None
            P.barrier()
            P.flush()

    if "attn" in phases:
        with contextlib.ExitStack() as es:
            acc = sb(es, "acc", [128, 2, S], F32)
            lds = [[Buf(sb(es, "ld%d_%d" % (i, j), [128, S], BF16)) for j in range(3)] for i in range(2)]
            vbp = RR([sb(es, "vb%d" % i, [128, 32, 128], BF16) for i in range(2)])
            ptp = RR([sb(es, "pt%d" % i, [128, 2, 128], BF16) for i in range(3)])
            rl = sb(es, "rl", [128, S], F32)
            ohp = RR([sb(es, "oh%d" % i, [128, S], BF16) for i in range(2)])
            psS = RR([ps[:, i, 0:256].rearrange("p (a b) -> p a b", b=128) for i in range(3)], excl=True)
            psO = RR([ps[:, 3 + i, 0:256].rearrange("p (a b) -> p a b", b=128) for i in range(3)], excl=True)
            psTp = RR([ps[:, 6 + i, :].rearrange("p (a b) -> p a b", b=128) for i in range(2)], excl=True)
            B_acc, B_rl = Buf(), Buf()
            scale = float(DH ** -0.5)
            NHEADS = test.get("nheads", H)
            li = 0
            items = []
            state = {"first_acc": True, "li": 0}

            def make_group(h, g):
                d = DILS[g]
                nb = 32 // d
                span = 128 * d
                nq0 = HALO0 // span
                nk0 = max(nq0 - 1, 0)
                ctx = {}

                def pre1():
                    lb = lds[state["li"] % 2]
                    state["li"] += 1
                    for j, src in enumerate((QT[g], KT[g], VT[g])):
                        rdb = [dbuf(("qkv", j, g, h, st)) for st in range(NST)] if "qkv" in phases else []
                        rdb = [b_ for b_ in rdb if b_.wr]
                        l0_ = HALO0 if j == 0 else nk0 * span
                        P.dma("sp", lb[j].ap[:, l0_:S], src[h * 128:(h + 1) * 128, l0_:S], rd=rdb, wr=(lb[j],))
                    Vh = lb[2].ap
                    vb = vbp.next()
                    need_bi = [r * nb + n for r in range(d) for n in range(nk0, nb)]
                    first_vb = True
                    for q4 in range(0, len(need_bi), 4):
                        grp = need_bi[q4:q4 + 4]
                        pT_ = psTp.next()
                        inss = []
                        for j, bi in enumerate(grp):
                            r, n = bi // nb, bi % nb
                            s0 = span * n + r
                            inss.append(mm(pT_.ap[:, j, :], Vh[:, s0:s0 + 127 * d + 1:d], identb[:], True, True))
                        P.op("pe", inss, rd=(lb[2], B_id), wr=(pT_,))
                        for j, bi in enumerate(grp):
                            P.op("act", I("activation", out=vb.ap[:, bi, :], in_=pT_.ap[:, j, :], func=AF.Copy),
                                 rd=(pT_,), wr=(vb,), acc=(not first_vb))
                            first_vb = False
                    ctx["lb"], ctx["vb"] = lb, vb

                first = True
                for r in range(d):
                    for n in range(nq0, nb):
                        bi = r * nb + n
                        s0 = span * n + r
                        i_lo = 0
                        if s0 < HALO0:
                            i_lo = -(-(HALO0 - s0) // d)
                        nq = 128 - i_lo
                        if nq <= 0:
                            continue

                        def stage1(bi=bi, s0=s0, i_lo=i_lo, nq=nq, n=n):
                            lb = ctx["lb"]
                            Qh, Kh = lb[0].ap, lb[1].ap
                            qsl = slice(s0 + i_lo * d, s0 + 127 * d + 1, d)
                            cur = slice(s0, s0 + 127 * d + 1, d)
                            prv = slice(s0 - span, s0 - span + 127 * d + 1, d)
                            has_prev = n > 0
                            cur_pad = (s0 + 127 * d) < OWN
                            prv_pad = has_prev and (s0 - span + 127 * d) < OWN
                            assert (s0 >= OWN) or cur_pad
                            Sb = psS.next()
                            bc = biasp if cur_pad else biasb
                            inss = [mm(Sb.ap[:, 1, 0:nq], Kh[:, cur], Qh[:, qsl], True, False),
                                    mm(Sb.ap[:, 1, 0:nq], identb[:], bc[:, 1, i_lo:128], False, True)]
                            if has_prev:
                                bp_ = biasp if prv_pad else biasb
                                inss += [mm(Sb.ap[:, 0, 0:nq], Kh[:, prv], Qh[:, qsl], True, False),
                                         mm(Sb.ap[:, 0, 0:nq], identb[:], bp_[:, 0, i_lo:128], False, True)]
                            P.op("pe", inss, rd=(lb[0], lb[1], B_id, B_mask, B_maskp), wr=(Sb,))
                            pt = ptp.next()
                            lo = 0 if has_prev else 1
                            P.op("act", I("activation", out=pt.ap[:, lo:2, 0:nq], in_=Sb.ap[:, lo:2, 0:nq], func=AF.Exp,
                                          scale=scale), rd=(Sb,), wr=(pt,))
                            return pt

                        def stage2(pt, bi=bi, s0=s0, i_lo=i_lo, nq=nq, n=n):
                            vb = ctx["vb"]
                            qsl = slice(s0 + i_lo * d, s0 + 127 * d + 1, d)
                            has_prev = n > 0
                            Ob = psO.next()
                            inss = []
                            for half in range(2):
                                lc = vb.ap[:, bi, :] if half == 0 else onesb[:]
                                if has_prev:
                                    lp = vb.ap[:, bi - 1, :] if half == 0 else onesb[:]
                                    inss.append(mm(Ob.ap[:, half, 0:nq], lp, pt.ap[:, 0, 0:nq], True, False))
                                inss.append(mm(Ob.ap[:, half, 0:nq], lc, pt.ap[:, 1, 0:nq], not has_prev, True))
                            P.op("pe", inss, rd=(pt, vb, B_ob), wr=(Ob,))
                            P.op("dve", I("tensor_tensor", out=acc[:, :, qsl], in0=Ob.ap[:, :, 0:nq], in1=acc[:, :, qsl],
                                          op=ALU.add), rd=(Ob,), wr=(B_acc,), acc=(not state["first_acc"]))
                            state["first_acc"] = False

                        items.append([pre1 if first else None, stage1, stage2, None])
                        first = False

            def make_fin(h):
                def fin():
                    oh = ohp.next()
                    P.op("dve", I("tensor_scalar", out=rl[:, HALO0:S], in0=acc[:, 1, HALO0:S], scalar1=1e-30, scalar2=None,
                                  op0=ALU.add), rd=(B_acc,), wr=(B_rl,))
                    P.op("dve", I("reciprocal", out=rl[:, HALO0:S], in_=rl[:, HALO0:S]), rd=(), wr=(B_rl,))
                    P.op("dve", I("tensor_tensor", out=oh.ap[:, HALO0:S], in0=acc[:, 0, HALO0:S], in1=rl[:, HALO0:S],
                                  op=ALU.mult), rd=(B_acc, B_rl), wr=(oh,))
                    P.dma("sp", oT[h * 128:(h + 1) * 128, HALO0:S], oh.ap[:, HALO0:S], rd=(oh,), wr=(dbuf(("oT", h)),))
                    P.op("pool", I("memset", ap=acc[:, :, HALO0:S], constant=0.0), wr=(B_acc,))
                    state["first_acc"] = True
                return fin

            for h in range(NHEADS):
                for g in range(3):
                    make_group(h, g)
                    k0 = len(items)
                items[-1][3] = make_fin(h)
            P.op("pool", I("memset", ap=acc[:, :, HALO0:S], constant=0.0), wr=(B_acc,))
            LOOK = 2
            pend = {}
            for i in range(len(items) + LOOK):
                if i < len(items):
                    if items[i][0] is not None:
                        items[i][0]()
                    pend[i] = items[i][1]()
                j = i - LOOK
                if j >= 0:
                    items[j][2](pend.pop(j))
                    if items[j][3] is not None:
                        items[j][3]()
            P.barrier()
            P.flush()

    if "l0" in phases or "l1" in phases:
        with contextlib.ExitStack() as es:
            z = sb(es, "z", [128, KC, 2, 512], F32)
            zb = [[Buf(z[:, c, t, :]) for t in range(2)] for c in range(KC)]
            zc = [(zb[c][0], zb[c][1]) for c in range(KC)]
            xb = sb(es, "xb3", [128, KC, 2, 512], BF16)
            B_xb = Buf()
            hbuf = sb(es, "hbuf", [128, KC, 2, 512], BF16)
            hb = [Buf(hbuf[:, i]) for i in range(KC)]
            w13 = RR([sb(es, "w13_%d" % i, [128, KC, 128], BF16) for i in range(4)])
            w2p = RR([sb(es, "w2_%d" % i, [128, HF, 128], BF16) for i in range(2)])
            sgp = RR([sb(es, "sg%d" % i, [128, 2, 512], F32) for i in range(2)])
            gbp = RR([sb(es, "gb%d" % i, [128, 2, 512], F32) for i in range(1)])
            sqp = RR([sb(es, "sq%d" % i, [128, 512], F32) for i in range(2)])
            mean_t = sb(es, "mean_t", [128, 512], F32)
            var_t = sb(es, "var_t", [128, 512], F32)
            rstd_t = sb(es, "rstd_t", [128, 512], F32)
            nmr_t = sb(es, "nmr_t", [128, 512], F32)
            B_mean, B_var, B_rstd, B_nmr = Buf(), Buf(), Buf(), Buf()
            utail = sb(es, "utail", [128, KC, 16], F32)
            B_ut = [Buf() for _ in range(KC)]
            ubt = [sb(es, "ub%d" % i, [128, 16 + ST], F32) for i in range(2)]
            uat = [sb(es, "ua%d" % i, [128, 16 + ST], F32) for i in range(2)]
            ubp = RR(ubt)
            uap = RR(uat)

            def v8(t):
                return t[0:8, 0:ST].rearrange("p (a b) -> p a b", b=512)

            L8, R8, C8, E8 = v8(ubt[0]), v8(ubt[1]), v8(uat[0]), v8(uat[1])
            G8 = sb(es, "G8", [8, 2, 512], F32)
            B_L8, B_R8, B_C8, B_E8, B_G8 = Buf(), Buf(), Buf(), Buf(), Buf()
            ps2 = RR([ps[:, 2 * i:2 * i + 2, :] for i in range(4)], excl=True)

            geo = {"nt": 2, "tw": 512}

            def W(ap):
                return ap[:, 0:geo["nt"], 0:geo["tw"]]

            def Wt(ap, t):
                return ap[:, t, 0:geo["tw"]]

            def Wf(ap):
                return ap[:, 0:geo["tw"]]

            def cols(ap2d):
                return ap2d.rearrange("p (a b) -> p a b", b=geo["tw"])

            def cols3(ap3d):
                return ap3d.rearrange("p c (a b) -> p c a b", b=geo["tw"])

            def mm2(psb, lhs_list, rhs_of, rd):
                n = len(lhs_list)
                P.op("pe", [mm(Wt(psb.ap, t), lhs_list[k], rhs_of(k, t), k == 0, k == n - 1)
                            for t in range(geo["nt"]) for k in range(n)], rd=rd, wr=(psb,))

            def xb_rhs(k, t):
                return Wt(xb[:, k], t)

            def load_xb_from_z():
                for c in range(KC):
                    if c % 2 == 0:
                        P.op("act", I("activation", out=W(xb[:, c]), in_=W(z[:, c]), func=AF.Copy), rd=zc[c], wr=(B_xb,), acc=True)
                    else:
                        P.op("pool", I("tensor_copy", out=W(xb[:, c]), in_=W(z[:, c])), rd=zc[c], wr=(B_xb,), acc=True)

            def layer_norm(vg, vb_):
                for t in range(geo["nt"]):
                    pss = ps2.next()
                    P.op("pe", [mm(Wt(pss.ap, 0), ones32[:], Wt(z[:, c], t), c == 0, c == KC - 1) for c in range(KC)],
                         rd=[zb[c][t] for c in range(KC)] + [B_o32], wr=(pss,))
                    for c in range(KC):
                        sq = sqp.next()
                        P.op("act", I("activation", out=Wf(sq.ap), in_=Wt(z[:, c], t), func=AF.Square), rd=(zb[c][t],), wr=(sq,))
                        P.op("pe", mm(Wt(pss.ap, 1), ones32[:], Wf(sq.ap), c == 0, c == KC - 1), rd=(sq,), wr=(pss,), acc=True)
                    P.op("dve", I("tensor_scalar", out=Wf(mean_t), in0=Wt(pss.ap, 0), scalar1=1.0 / D, scalar2=None,
                                  op0=ALU.mult), rd=(pss,), wr=(B_mean,))
                    P.op("dve", I("tensor_tensor", out=Wf(var_t), in0=Wf(mean_t), in1=Wf(mean_t), op=ALU.mult),
                         rd=(B_mean,), wr=(B_var,))
                    P.op("dve", I("scalar_tensor_tensor", out=Wf(var_t), in0=Wt(pss.ap, 1), scalar=1.0 / D, in1=Wf(var_t),
                                  op0=ALU.mult, op1=ALU.subtract), rd=(pss,), wr=(B_var,))
                    P.op("dve", I("tensor_scalar", out=Wf(var_t), in0=Wf(var_t), scalar1=EPS, scalar2=None,
                                  op0=ALU.add), rd=(), wr=(B_var,))
                    P.op("act", I("activation", out=Wf(rstd_t), in_=Wf(var_t), func=AF.Sqrt), rd=(B_var,), wr=(B_rstd,))
                    P.op("dve", I("reciprocal", out=Wf(rstd_t), in_=Wf(rstd_t)), rd=(), wr=(B_rstd,))
                    P.op("dve", I("scalar_tensor_tensor", out=Wf(nmr_t), in0=Wf(mean_t), scalar=-1.0, in1=Wf(rstd_t),
                                  op0=ALU.mult, op1=ALU.mult), rd=(B_mean, B_rstd), wr=(B_nmr,))
                    for c in range(KC):
                        zz = Wt(z[:, c], t)
                        P.op("dve", I("tensor_tensor", out=zz, in0=zz, in1=Wf(rstd_t), op=ALU.mult),
                             rd=(B_rstd,), wr=(zb[c][t],))
                        P.op("pool", I("tensor_tensor", out=zz, in0=zz, in1=Wf(nmr_t), op=ALU.add),
                             rd=(B_nmr,), wr=(zb[c][t],))
                        P.op("act", I("activation", out=zz, in_=zz, func=AF.Identity,
                                      scale=vecs[:, vg, c:c + 1], bias=vecs[:, vb_, c:c + 1]),
                             rd=(B_vecs,), wr=(zb[c][t],))

            def gemm_resid(wdram, src_bufs, rhs_of, nk):
                reqs = [(wdram[:, :, oc * 128:(oc + 1) * 128], (lambda ap, nk=nk: ap[:, :nk, :])) for oc in range(KC)]
                stream = Stream(P, w13, reqs, depth=2)
                for oc in range(KC):
                    wb = stream.get(oc)
                    psb = ps2.next()
                    mm2(psb, [wb.ap[:, k, :] for k in range(nk)], rhs_of, rd=[wb] + list(src_bufs))
                    P.op("dve", I("scalar_tensor_tensor", out=W(z[:, oc]), in0=W(z[:, oc]), scalar=ALPHA, in1=W(psb.ap),
                                  op0=ALU.mult, op1=ALU.add), rd=(psb,), wr=zc[oc])

            def ffn(experts):
                first = True
                plan = []
                for (w1v, w3v, w2v, ge) in experts:
                    for part in range(NSPLIT):
                        reqs = []
                        for fi in range(HF):
                            f = part * HF + fi
                            reqs.append((w1v[:, :, f * 128:(f + 1) * 128], (lambda ap: ap[:])))
                            reqs.append((w3v[:, :, f * 128:(f + 1) * 128], (lambda ap: ap[:])))
                        s13 = Stream(P, w13, reqs, depth=2)
                        reqs2 = [(w2v[:, part * HF:(part + 1) * HF, oc * 128:(oc + 1) * 128], (lambda ap: ap[:]))
                                 for oc in range(KC)]
                        s2 = Stream(P, w2p, reqs2, depth=1)
                        plan.append((ge, part, s13, s2))
                plan[0][2].prefetch(4)
                gb = None
                for pi, (ge, part, stream, stream2) in enumerate(plan):
                    if ge is not None and part == 0:
                        gb = gbp.next()
                        psb = ps2.next()
                        P.op("pe", [mm(Wt(psb.ap, t), selb[:, ge, :], Wt(G8, t), True, True) for t in range(geo["nt"])],
                             rd=(B_G8, B_selb), wr=(psb,))
                        P.op("act", I("activation", out=W(gb.ap), in_=W(psb.ap), func=AF.Copy), rd=(psb,), wr=(gb,))
                    stream2.prefetch(2)
                    for fi in range(HF):
                        wb1 = stream.get(2 * fi)
                        wb3 = stream.get(2 * fi + 1)
                        pg = ps2.next()
                        pu = ps2.next()
                        mm2(pg, [wb1.ap[:, k, :] for k in range(KC)], xb_rhs, rd=(wb1, B_xb))
                        mm2(pu, [wb3.ap[:, k, :] for k in range(KC)], xb_rhs, rd=(wb3, B_xb))
                        sg = sgp.next()
                        P.op("act", I("activation", out=W(sg.ap), in_=W(pg.ap), func=AF.Silu), rd=(pg,), wr=(sg,))
                        if ge is not None:
                            P.op("dve", I("tensor_tensor", out=W(sg.ap), in0=W(pu.ap), in1=W(sg.ap), op=ALU.mult),
                                 rd=(pu,), wr=(sg,))
                            P.op("pool", I("tensor_tensor", out=W(hbuf[:, fi]), in0=W(sg.ap), in1=W(gb.ap), op=ALU.mult),
                                 rd=(sg, gb), wr=(hb[fi],))
                        else:
                            P.op("dve", I("tensor_tensor", out=W(hbuf[:, fi]), in0=W(pu.ap), in1=W(sg.ap), op=ALU.mult),
                                 rd=(pu, sg), wr=(hb[fi],))
                    if pi + 1 < len(plan):
                        plan[pi + 1][2].prefetch(4)
                    for oc in range(KC):
                        wb = stream2.get(oc)
                        py = ps2.next()
                        nt_ = geo["nt"]
                        if oc == 0:
                            P.op("pe", [mm(Wt(py.ap, t), wb.ap[:, k, :], Wt(hbuf[:, k], t), k == 0, False)
                                        for t in range(nt_) for k in range(HF - 1)], rd=[wb] + hb[:HF - 1], wr=(py,))
                            P.op("pe", [mm(Wt(py.ap, t), wb.ap[:, HF - 1, :], Wt(hbuf[:, HF - 1], t), False, True)
                                        for t in range(nt_)], rd=[wb, hb[HF - 1]], wr=(py,), acc=True)
                        else:
                            mm2(py, [wb.ap[:, k, :] for k in range(HF)], (lambda k, t: Wt(hbuf[:, k], t)),
                                rd=[wb] + hb[:HF])
                        if first:
                            P.op("dve", I("scalar_tensor_tensor", out=W(z[:, oc]), in0=W(z[:, oc]), scalar=ALPHA,
                                          in1=W(py.ap), op0=ALU.mult, op1=ALU.add), rd=(py,), wr=zc[oc])
                        else:
                            P.op("dve", I("tensor_tensor", out=W(z[:, oc]), in0=W(py.ap), in1=W(z[:, oc]), op=ALU.add),
                                 rd=(py,), wr=zc[oc])
                    first = False

            def ple(layer, p0, vbias, out_ap_of, out_key):
                ntok = geo["nt"] * geo["tw"]
                load_xb_from_z()
                pb = hbuf[:, 0:2]
                P.dma("pool", pb[:, :, 0:geo["nt"], 0:geo["tw"]], cols3(chunkview(pT[layer])[:, :, p0:p0 + ntok]),
                      wr=(hb[0], hb[1]))
                wgv = chunkview(ple_w_gate[layer])
                wpv = chunkview(ple_w_proj[layer])
                reqs = []
                for oc in range(KC):
                    reqs.append((wgv[:, :, oc * 128:(oc + 1) * 128], (lambda ap: ap[:])))
                    reqs.append((wpv[:, :, oc * 128:(oc + 1) * 128], (lambda ap: ap[:, 0:2, :])))
                stream = Stream(P, w13, reqs, depth=2)
                for oc in range(KC):
                    wg = stream.get(2 * oc)
                    wp = stream.get(2 * oc + 1)
                    pg = ps2.next()
                    pp = ps2.next()
                    mm2(pg, [wg.ap[:, k, :] for k in range(KC)], xb_rhs, rd=(wg, B_xb))
                    mm2(pp, [wp.ap[:, k, :] for k in range(2)], (lambda k, t: Wt(pb[:, k], t)), rd=(wp, hb[0], hb[1]))
                    sg = sgp.next()
                    P.op("act", I("activation", out=W(sg.ap), in_=W(pg.ap), func=AF.Sigmoid,
                                  bias=vecs[:, vbias, oc:oc + 1], scale=1.0), rd=(pg, B_vecs), wr=(sg,))
                    P.op("dve", I("tensor_tensor", out=W(sg.ap), in0=W(pp.ap), in1=W(sg.ap), op=ALU.mult), rd=(pp,), wr=(sg,))
                    P.op("pool", I("tensor_tensor", out=W(z[:, oc]), in0=W(z[:, oc]), in1=W(sg.ap), op=ALU.add),
                         rd=(sg,), wr=zc[oc])
                    if out_ap_of is not None:
                        P.dma("sp", cols(out_ap_of(oc)), W(z[:, oc]), rd=zc[oc], wr=(dbuf((out_key, oc, p0)),))

            def dump(name, e0):
                if name in dbg:
                    ntok = geo["nt"] * geo["tw"]
                    for oc in range(KC):
                        P.dma("sp", cols(dbg[name][oc * 128:(oc + 1) * 128, e0:e0 + ntok]), W(z[:, oc]),
                              rd=zc[oc], wr=(dbuf((name, oc, e0)),))

            P.op("pool", I("memset", ap=utail[:], constant=0.0), wr=B_ut)
            TILES = test.get("tiles", [(HALO0, 1, 128), (OWN, 2, 512), (OWN + ST, 2, 512)])
            for (e0, nt_, tw_) in TILES:
                geo["nt"], geo["tw"] = nt_, tw_
                ntok = nt_ * tw_
                is_halo = e0 < OWN
                if "l0" in phases:
                    for c in range(KC):
                        P.dma("sp", W(z[:, c]), cols(xT[c * 128:(c + 1) * 128, e0:e0 + ntok]), wr=zc[c])
                    P.dma("sp", xb[:, :, 0:nt_, 0:tw_], cols3(chunkview(oT.ap())[:, :, e0:e0 + ntok]),
                          rd=[dbuf(("oT", h)) for h in range(H)] if "attn" in phases else (), wr=(B_xb,))
                    gemm_resid(chunkview(w_o.ap()), (B_xb,), xb_rhs, KC)
                    layer_norm(V_LN_MIX_G0, V_LN_MIX_B0)
                    dump("x1", e0)
                    load_xb_from_z()
                    ffn([(chunkview(ffn_w1.ap()), chunkview(ffn_w3.ap()), chunkview(ffn_w2.ap()), None)])
                    layer_norm(V_LN_FFN_G0, V_LN_FFN_B0)
                    dump("x2", e0)
                    ple(0, e0 - HALO0, V_PLE_B0, (lambda oc, e0=e0, ntok=ntok: x3T[oc * 128:(oc + 1) * 128, e0:e0 + ntok]), "x3")
                if "l1" in phases:
                    if "l0" not in phases:
                        for c in range(KC):
                            P.dma("sp", W(z[:, c]), cols(x3T[c * 128:(c + 1) * 128, e0:e0 + ntok]), wr=zc[c])
                    load_xb_from_z()
                    winv = chunkview(pool_w_in.ap())
                    stream = Stream(P, w13, [(winv[:, :, oc * 128:(oc + 1) * 128], (lambda ap: ap[:])) for oc in range(KC)],
                                    depth=2)
                    for oc in range(KC):
                        wb = stream.get(oc)
                        pu = ps2.next()
                        mm2(pu, [wb.ap[:, k, :] for k in range(KC)], xb_rhs, rd=(wb, B_xb))
                        ub = ubp.next()
                        gi = oc // 4
                        if is_halo:
                            P.op("act", I("activation", out=cols(ub.ap[:, 16:16 + ntok]), in_=W(pu.ap), func=AF.Copy),
                                 rd=(pu,), wr=(ub,))
                            P.op("pool", I("tensor_tensor", out=utail[:, oc, :], in0=ub.ap[:, ntok:ntok + 16], in1=hflag[:],
                                           op=ALU.mult), rd=(ub, B_hflag), wr=(B_ut[oc],))
                            continue
                        ua = uap.next()
                        P.op("pool", I("tensor_copy", out=ub.ap[:, 0:16], in_=utail[:, oc, :]), rd=(B_ut[oc],), wr=(ub,))
                        P.op("act", I("activation", out=cols(ub.ap[:, 16:16 + ntok]), in_=W(pu.ap), func=AF.Copy),
                             rd=(pu,), wr=(ub,), acc=True)
                        P.op("pool", I("tensor_copy", out=utail[:, oc, :], in_=ub.ap[:, ntok:ntok + 16]), rd=(ub,), wr=(B_ut[oc],))
                        P.op("dve", I("tensor_tensor", out=ua.ap[:, 1:16 + ntok], in0=ub.ap[:, 1:16 + ntok],
                                      in1=ub.ap[:, 0:15 + ntok], op=ALU.add), rd=(ub,), wr=(ua,))
                        sh, lo = 2, 1
                        for _ in range(gi):
                            lo2 = lo + sh
                            ua2 = uap.next()
                            P.op("dve", I("tensor_tensor", out=ua2.ap[:, lo2:16 + ntok], in0=ua.ap[:, lo2:16 + ntok],
                                          in1=ua.ap[:, lo2 - sh:16 + ntok - sh], op=ALU.add), rd=(ua,), wr=(ua2,))
                            ua, lo, sh = ua2, lo2, sh * 2
                        w = POOLW[gi]
                        if e0 == OWN:
                            P.op("dve", I("tensor_tensor", out=ua.ap[:, 16:32], in0=ua.ap[:, 16:32], in1=corr[:, gi, :],
                                          op=ALU.mult), rd=(B_corr,), wr=(ua,))
                        P.op("dve", I("scalar_tensor_tensor", out=W(hbuf[:, oc]), in0=cols(ua.ap[:, 16:16 + ntok]), scalar=1.0 / w,
                                      in1=cols(ub.ap[:, 16:16 + ntok]), op0=ALU.mult, op1=ALU.subtract),
                             rd=(ua, ub), wr=(hb[oc],))
                    if is_halo:
                        continue
                    reqs = []
                    for oc in range(KC):
                        gi, ol = oc // 4, oc % 4
                        reqs.append((chunkview(pool_w_group[gi])[:, :, ol * 128:(ol + 1) * 128], (lambda ap: ap[:, 0:4, :])))
                    stream = Stream(P, w13, reqs, depth=2)
                    for oc in range(KC):
                        gi = oc // 4
                        wb = stream.get(oc)
                        py = ps2.next()
                        mm2(py, [wb.ap[:, k, :] for k in range(4)], (lambda k, t, gi=gi: Wt(hbuf[:, gi * 4 + k], t)),
                            rd=[wb] + hb[gi * 4:gi * 4 + 4])
                        P.op("act", I("activation", out=W(xb[:, oc]), in_=W(py.ap), func=AF.Identity,
                                      scale=vecs[:, V_POOL_SCALE, oc:oc + 1], bias=0.0),
                             rd=(py, B_vecs), wr=(B_xb,), acc=(oc > 0))
                    gemm_resid(chunkview(pool_w_o.ap()), (B_xb,), xb_rhs, KC)
                    layer_norm(V_LN_MIX_G1, V_LN_MIX_B1)
                    dump("x4", e0)
                    pl = ps2.next()
                    P.op("pe", [mm(Wt(pl.ap[0:8], t), wr32[:, c, :], Wt(z[:, c], t), c == 0, c == KC - 1)
                                for t in range(nt_) for c in range(KC)],
                         rd=[zb[c][t] for c in range(KC) for t in range(2)] + [B_wr], wr=(pl,))
                    P.op("act", I("activation", out=W(L8), in_=W(pl.ap[0:8]), func=AF.Copy), rd=(pl,), wr=(B_L8,))
                    P.op("act", I("activation", out=W(E8), in_=W(L8), func=AF.Exp), rd=(B_L8,), wr=(B_E8,))
                    P.op("dve", I("memset", ap=W(R8), constant=0.0), wr=(B_R8,))
                    for e2 in range(NE):
                        pb_ = ps2.next()
                        P.op("pe", [mm(Wt(pb_.ap[0:8], t), selb[:, e2, 0:8], Wt(L8, t), True, True) for t in range(nt_)],
                             rd=(B_L8, B_selb), wr=(pb_,))
                        P.op("dve", I("tensor_tensor", out=W(C8), in0=W(pb_.ap[0:8]), in1=W(L8), op=ALU.is_gt),
                             rd=(pb_, B_L8), wr=(B_C8,))
                        P.op("dve", I("tensor_tensor", out=W(R8), in0=W(R8), in1=W(C8), op=ALU.add), rd=(B_C8,), wr=(B_R8,))
                    P.op("dve", I("scalar_tensor_tensor", out=W(E8), in0=W(R8), scalar=1.5, in1=W(E8), op0=ALU.is_lt,
                                  op1=ALU.mult), rd=(B_R8,), wr=(B_E8,))
                    pd = ps2.next()
                    P.op("pe", [mm(Wt(pd.ap[0:8], t), ones32[0:8, 0:8], Wt(E8, t), True, True) for t in range(nt_)],
                         rd=(B_E8, B_o32), wr=(pd,))
                    P.op("dve", I("reciprocal", out=W(C8), in_=W(pd.ap[0:8])), rd=(pd,), wr=(B_C8,))
                    P.op("dve", I("tensor_tensor", out=W(G8), in0=W(E8), in1=W(C8), op=ALU.mult), rd=(B_E8, B_C8), wr=(B_G8,))
                    load_xb_from_z()
                    ffn([(chunkview(moe_w1[e_]), chunkview(moe_w3[e_]), chunkview(moe_w2[e_]), e_) for e_ in range(ne_run)])
                    layer_norm(V_LN_FFN_G1, V_LN_FFN_B1)
                    dump("x5", e0)
                    ple(1, e0 - HALO0, V_PLE_B1,
                        (lambda oc, e0=e0, ntok=ntok: outT[oc * 128:(oc + 1) * 128, e0 - OWN:e0 - OWN + ntok]), "out")
            P.barrier()
            P.flush()
    es0.close()
    P.stack.close()
    build.last_ninstr = P.ninstr
    return nc


def host_consts(half):
    hd = DH // 2
    inv = (10000.0 ** (-np.arange(hd, dtype=np.float32) / hd)).astype(np.float32)
    pos = (np.arange(S) - (0 if half == 1 else OWN)).astype(np.float32)
    ang = pos[:, None] * inv[None, :]
    c = np.cos(ang).astype(np.float32).T
    s = np.sin(ang).astype(np.float32).T
    cosT = np.concatenate([c, c], 0)
    sinT = np.concatenate([-s, s], 0)
    rm = np.zeros((128, 128), np.float32)
    for m in range(128):
        rm[(m + 64) % 128, m] = 1.0
    i = np.arange(128)
    mask = np.zeros((128, 2, 128), np.float32)
    mask[:, 0, :] = (i[:, None] >= i[None, :])
    mask[:, 1, :] = (i[:, None] <= i[None, :])
    NEG = -30000.0
    mask = np.where(mask > 0, 0.0, NEG).astype(np.float32)
    maskp = mask.copy() if half == 1 else np.full_like(mask, NEG)
    hflag = np.full((128, 16), 1.0 if half == 1 else 0.0, np.float32)
    ident = np.eye(128, dtype=np.float32)
    selb = np.zeros((8, NE, 128), np.float32)
    for e in range(NE):
        selb[e, e, :] = 1.0
    corr = np.ones((128, 4, 16), np.float32)
    if half == 0:
        for gi, w in enumerate(POOLW):
            t = np.arange(16)
            corr[:, gi, :] = (w / np.minimum(t + 1, w)).astype(np.float32)[None, :]
    return dict(cosT=np.ascontiguousarray(cosT), sinT=np.ascontiguousarray(sinT), rmat=rm, maskT=mask, maskP=maskp,
                hflag=hflag, ident=ident, selb=selb, corr=corr)


def pack_vecs(inp):
    def lay(v):
        return np.asarray(v, np.float32).reshape(KC, 128).T
    vs = [inp["ln_mix_g"][0], inp["ln_mix_b"][0], inp["ln_ffn_g"][0], inp["ln_ffn_b"][0],
          inp["ln_mix_g"][1], inp["ln_mix_b"][1], inp["ln_ffn_g"][1], inp["ln_ffn_b"][1],
          inp["ple_b_gate"][0], inp["ple_b_gate"][1], np.asarray(inp["pool_scale"][0]).reshape(-1)]
    return np.ascontiguousarray(np.stack([lay(v) for v in vs], axis=1))


def shared_inputs(inp):
    f = lambda a: np.ascontiguousarray(np.asarray(a, np.float32))
    m = dict(
        attn_w_qkv=f(inp["attn_w_qkv"][0]), attn_w_o=f(inp["attn_w_o"][0]),
        pool_w_in=f(inp["pool_w_in"][0]), pool_w_group=f(inp["pool_w_group"][0]), pool_w_o=f(inp["pool_w_o"][0]),
        ffn_w1=f(inp["ffn_w1"][0]), ffn_w3=f(inp["ffn_w3"][0]), ffn_w2=f(inp["ffn_w2"][0]),
        moe_router=f(inp["moe_router"][0]), moe_w1=f(inp["moe_w1"][0]), moe_w3=f(inp["moe_w3"][0]),
        moe_w2=f(inp["moe_w2"][0]), ple_w_proj=f(inp["ple_w_proj"]), ple_w_gate=f(inp["ple_w_gate"]),
        vecs=pack_vecs(inp),
    )
    return m


def core_inputs(x, p, b, half):
    m = host_consts(half)
    xT = np.zeros((D, S), np.float32)
    pT = np.zeros((2, 256, NQ), np.float32)
    if half == 1:
        xT[:] = x[b].T
        pT[:] = np.transpose(p[:, b, HALO0:S], (0, 2, 1))
    else:
        xT[:, OWN:] = x[b, 0:OWN].T
        pT[:, :, OWN - HALO0:] = np.transpose(p[:, b, 0:OWN], (0, 2, 1))
    m["xT"] = xT
    m["pT"] = pT
    return m


def kernel(**inp):
    ncores = 8
    x = np.asarray(inp["x"], np.float32)
    p = np.asarray(inp["p"], np.float32)
    shared = shared_inputs(inp)
    in_maps = []
    for c in range(ncores):
        m = dict(shared)
        m.update(core_inputs(x, p, c // 2, c % 2))
        in_maps.append(m)
    nc = build(ncores)
    res = run_bass_kernel_spmd(nc, in_maps, core_ids=list(range(ncores)))
    out = np.zeros((4, S, D), np.float32)
    for c in range(ncores):
        b, half = c // 2, c % 2
        out[b, half * OWN:(half + 1) * OWN, :] = res.results[c]["outT"].T
    return out
```

```python
import contextlib
import numpy as np
import concourse.bass as bass
import concourse.mybir as mybir
from concourse.bass_utils import run_bass_kernel_spmd

F32 = mybir.dt.float32
BF16 = mybir.dt.bfloat16
AF = mybir.ActivationFunctionType
ALU = mybir.AluOpType

S = 4096
OWN = 2048
HALO0 = 1920
NQ = S - HALO0
D = 2048
H = 16
DH = 128
DFF = 5632
NE = 8
ST = 1024
NST = S // ST
KC = D // 128
FC = DFF // 128
NSPLIT = 4
HF = FC // NSPLIT
ALPHA = float((2 * 2) ** 0.25)
EPS = 1e-5
DILS = (1, 4, 16)
POOLW = (2, 4, 8, 16)
ENGS = ("pe", "act", "dve", "pool", "sp")

V_LN_MIX_G0, V_LN_MIX_B0, V_LN_FFN_G0, V_LN_FFN_B0 = 0, 1, 2, 3
V_LN_MIX_G1, V_LN_MIX_B1, V_LN_FFN_G1, V_LN_FFN_B1 = 4, 5, 6, 7
V_PLE_B0, V_PLE_B1, V_POOL_SCALE = 8, 9, 10
NV = 11


def I(name, **kw):
    return (name, kw)


class Buf:
    def __init__(self, ap=None, excl=False):
        self.ap = ap
        self.wr = {}
        self.rd = {}
        self.excl = excl


class Prog:
    def __init__(self, nc):
        self.nc = nc
        self.q = {e: [] for e in ENGS}
        self.sems = {}
        self.cnt = {}
        self.stack = contextlib.ExitStack()
        self.dma_rr = {"pool": 0, "sp": 0}
        self.ndma_sems = {"pool": 12, "sp": 24}
        self.waited = {e: {} for e in ENGS}
        self.ninstr = 0

    def sem(self, name):
        if name not in self.sems:
            self.sems[name] = self.stack.enter_context(self.nc.semaphore(name))
            self.cnt[name] = 0
        return name

    def _deps(self, rd, wr, eng_sem=None, acc=False):
        deps = {}

        def add(d, skip=None):
            for s, v in d.items():
                if s == skip:
                    continue
                if deps.get(s, 0) < v:
                    deps[s] = v

        for b in rd:
            add(b.wr)
            if b.excl:
                add(b.rd, eng_sem)
        for b in wr:
            add(b.wr, eng_sem if acc else None)
            add(b.rd)
        return deps

    def _commit(self, tok, rd, wr, acc=False):
        s, v = tok
        for b in rd:
            if b.rd.get(s, 0) < v:
                b.rd[s] = v
        for b in wr:
            if acc:
                b.wr[s] = v
            else:
                b.wr = {s: v}
                b.rd = {}

    def op(self, eng, ins, rd=(), wr=(), acc=False):
        if isinstance(ins, tuple):
            ins = [ins]
        s = self.sem("p_" + eng)
        deps = self._deps(rd, wr, s, acc)
        self.cnt[s] += 1
        tok = (s, self.cnt[s])
        self.q[eng].append((ins, deps, tok, 1))
        self._commit(tok, rd, wr, acc)
        self.ninstr += len(ins)
        return tok

    def dma(self, eng, out, in_, rd=(), wr=()):
        i = self.dma_rr[eng]
        self.dma_rr[eng] = (i + 1) % self.ndma_sems[eng]
        s = self.sem("d_%s_%d" % (eng, i))
        deps = self._deps(rd, wr)
        if self.cnt[s] > 0:
            deps[s] = max(deps.get(s, 0), self.cnt[s])
        self.cnt[s] += 16
        tok = (s, self.cnt[s])
        self.q[eng].append(([I("dma_start", out=out, in_=in_)], deps, tok, 16))
        self._commit(tok, rd, wr)
        return tok

    def barrier(self):
        allv = {s: v for s, v in self.cnt.items() if v > 0}
        for e in ENGS:
            self.q[e].append(([], dict(allv), None, 0))

    def flush(self):
        nc = self.nc
        if not any(self.q[e] for e in ENGS):
            return
        with nc.Block() as block:
            decos = {"pe": block.tensor, "act": block.scalar, "dve": block.vector,
                     "pool": block.gpsimd, "sp": block.sync}
            for e in ENGS:
                items = self.q[e]
                if not items:
                    continue

                def body(engine, e=e, items=items):
                    waited = self.waited[e]
                    for inss, deps, tok, amt in items:
                        for s, v in deps.items():
                            if waited.get(s, 0) < v:
                                engine.wait_ge(self.sems[s], v)
                                waited[s] = v
                        last = None
                        for name, kw in inss:
                            last = getattr(engine, name)(**kw)
                        if tok is not None:
                            last.then_inc(self.sems[tok[0]], amt)

                decos[e](body)
        self.q = {e: [] for e in ENGS}


class RR:
    def __init__(self, aps, excl=False):
        self.bufs = [a if isinstance(a, Buf) else Buf(a, excl) for a in aps]
        self.i = 0

    def next(self):
        b = self.bufs[self.i]
        self.i = (self.i + 1) % len(self.bufs)
        return b


class Stream:
    def __init__(self, P, slots, reqs, depth=1):
        self.P = P
        self.slots = slots
        self.reqs = reqs
        self.issued = 0
        self.got = {}
        self.depth = depth

    def _issue_upto(self, last):
        while self.issued <= min(last, len(self.reqs) - 1):
            src, view = self.reqs[self.issued]
            b = self.slots.next()
            self.P.dma("pool", view(b.ap), src, rd=(), wr=(b,))
            self.got[self.issued] = b
            self.issued += 1

    def prefetch(self, n):
        self._issue_upto(n - 1)

    def get(self, i):
        self._issue_upto(i + self.depth)
        return self.got.pop(i)


def build(ncores=4, test=None):
    test = test or {}
    phases = test.get("phases", ("qkv", "attn", "l0", "l1"))
    ne_run = test.get("ne", NE)
    nc = bass.Bass("TRN2", target_bir_lowering=False)
    P = Prog(nc)

    def din(name, shape, dt=F32):
        if name in test.get("skip", ()):
            return nc.dram_tensor(name, [128, 128], dt)
        return nc.dram_tensor(name, list(shape), dt, kind="ExternalInput")

    def dscratch(name, shape, dt):
        role = test.get("roles", {}).get(name)
        if role == "in":
            return nc.dram_tensor(name, list(shape), dt, kind="ExternalInput")
        if role == "out":
            return nc.dram_tensor(name, list(shape), dt, kind="ExternalOutput")
        return nc.dram_tensor(name, list(shape), dt)

    xT = din("xT", [D, S])
    pT = din("pT", [2, 256, NQ])
    w_qkv = din("attn_w_qkv", [D, 3 * 3 * D])
    w_o = din("attn_w_o", [D, D])
    pool_w_in = din("pool_w_in", [D, D])
    pool_w_group = din("pool_w_group", [4, 512, 512])
    pool_w_o = din("pool_w_o", [D, D])
    ffn_w1 = din("ffn_w1", [D, DFF])
    ffn_w3 = din("ffn_w3", [D, DFF])
    ffn_w2 = din("ffn_w2", [DFF, D])
    moe_router = din("moe_router", [D, NE])
    moe_w1 = din("moe_w1", [NE, D, DFF])
    moe_w3 = din("moe_w3", [NE, D, DFF])
    moe_w2 = din("moe_w2", [NE, DFF, D])
    ple_w_proj = din("ple_w_proj", [2, 256, D])
    ple_w_gate = din("ple_w_gate", [2, D, D])
    vecs_d = din("vecs", [128, NV, KC])
    cos_d = din("cosT", [128, S])
    sin_d = din("sinT", [128, S])
    rmat_d = din("rmat", [128, 128])
    mask_d = din("maskT", [128, 2, 128])
    maskp_d = din("maskP", [128, 2, 128])
    hflag_d = din("hflag", [128, 16])
    ident_d = din("ident", [128, 128])
    selb_d = din("selb", [8, NE, 128])
    corr_d = din("corr", [128, 4, 16])
    outT = nc.dram_tensor("outT", [D, OWN], F32, kind="ExternalOutput")

    QT = [dscratch("QT%d" % g, [D, S], BF16) for g in range(3)]
    KT = [dscratch("KT%d" % g, [D, S], BF16) for g in range(3)]
    VT = [dscratch("VT%d" % g, [D, S], BF16) for g in range(3)]
    oT = dscratch("oT", [D, S], BF16)
    x3T = dscratch("x3T", [D, S], F32)
    dbg = {}
    for nm in test.get("dbg", ()):
        dbg[nm] = nc.dram_tensor("dbg_" + nm, [D, S], F32, kind="ExternalOutput")

    dbufs = {}

    def dbuf(key):
        if key not in dbufs:
            dbufs[key] = Buf()
        return dbufs[key]

    def chunkview(ap2d):
        return ap2d.rearrange("(c p) n -> p c n", p=128)

    def tt2(ap):
        return ap.rearrange("p (a b) -> p a b", b=512)

    def tt3(ap):
        return ap.rearrange("p c (a b) -> p c a b", b=512)

    es0 = contextlib.ExitStack()

    def sb(es, name, shape, dt):
        return es.enter_context(nc.sbuf_tensor(name, list(shape), dt))

    vecs = sb(es0, "vecs_sb", [128, NV, KC], F32)
    ones32 = sb(es0, "ones32", [128, 128], F32)
    onesb = sb(es0, "onesb", [128, 128], BF16)
    identb = sb(es0, "identb", [128, 128], BF16)
    rmat = sb(es0, "rmat_sb", [128, 128], BF16)
    biasb = sb(es0, "biasb", [128, 2, 128], BF16)
    biasp = sb(es0, "biasp", [128, 2, 128], BF16)
    hflag = sb(es0, "hflag_sb", [128, 16], F32)
    selb = sb(es0, "selb_sb", [8, NE, 128], F32)
    corr = sb(es0, "corr_sb", [128, 4, 16], F32)
    wr32 = sb(es0, "wr32", [128, KC, NE], F32)
    B_vecs, B_selb, B_corr, B_wr, B_id, B_rm, B_mask, B_o32, B_ob = (Buf() for _ in range(9))
    P.dma("sp", vecs[:], vecs_d.ap(), wr=(B_vecs,))
    P.dma("sp", selb[:], selb_d.ap(), wr=(B_selb,))
    P.dma("sp", corr[:], corr_d.ap(), wr=(B_corr,))
    P.dma("sp", wr32[:], chunkview(moe_router.ap()), wr=(B_wr,))
    P.dma("pool", identb[:], ident_d.ap(), wr=(B_id,))
    P.dma("pool", rmat[:], rmat_d.ap(), wr=(B_rm,))
    P.dma("pool", biasb[:], mask_d.ap(), wr=(B_mask,))
    B_maskp, B_hflag = Buf(), Buf()
    P.dma("pool", biasp[:], maskp_d.ap(), wr=(B_maskp,))
    P.dma("sp", hflag[:], hflag_d.ap(), wr=(B_hflag,))
    P.op("dve", I("memset", ap=ones32[:], constant=1.0), wr=(B_o32,))
    P.op("dve", I("memset", ap=onesb[:], constant=1.0), wr=(B_ob,))
    P.barrier()
    P.flush()

    ps = es0.enter_context(nc.psum_tensor("ps", [128, 8, 512], F32))

    def mm(out, lhsT, rhs, start, stop):
        return I("matmul", out=out, lhsT=lhsT, rhs=rhs, start=start, stop=stop)

    if "qkv" in phases:
        with contextlib.ExitStack() as es:
            xb = sb(es, "xb1", [128, KC, 2, 512], BF16)
            cos = sb(es, "cos", [128, 2 * NST, 512], F32)
            sin = sb(es, "sin", [128, 2 * NST, 512], F32)
            wsl = RR([sb(es, "wq%d" % i, [128, KC, 256], BF16) for i in range(2)])
            qbp = RR([sb(es, "qb%d" % i, [128, 2, 512], BF16) for i in range(2)])
            t1p = RR([sb(es, "t1_%d" % i, [128, 2, 512], F32) for i in range(2)])
            t2p = RR([sb(es, "t2_%d" % i, [128, 2, 512], F32) for i in range(2)])
            obp = RR([sb(es, "ob%d" % i, [128, 2, 512], BF16) for i in range(3)])
            psA = RR([ps[:, 0:2, :], ps[:, 4:6, :]], excl=True)
            psB = RR([ps[:, 2:4, :], ps[:, 6:8, :]], excl=True)
            B_cos, B_sin, B_xb = Buf(), Buf(), Buf()
            P.dma("sp", cos[:], cos_d.ap().rearrange("p (a b) -> p a b", b=512), wr=(B_cos,))
            P.dma("sp", sin[:], sin_d.ap().rearrange("p (a b) -> p a b", b=512), wr=(B_sin,))
            wqv = chunkview(w_qkv.ap())
            xTv = chunkview(xT.ap())
            WTS = test.get("wts", list(range(72)))
            for st in range(NST):
                P.dma("pool", xb[:], tt3(xTv[:, :, st * ST:(st + 1) * ST]), wr=(B_xb,))
                WT_ST = [wt for wt in WTS if st > 0 or (wt // 24 == 2 and (wt % 24) // 8 >= 1)]
                reqs = [(wqv[:, :, wt * 256:(wt + 1) * 256], (lambda ap: ap[:])) for wt in WT_ST]
                stream = Stream(P, wsl, reqs)
                pending = None
                for wi, wt in enumerate(WT_ST):
                    g = wt // 24
                    typ = (wt % 24) // 8
                    hp = wt % 8
                    wb = stream.get(wi)
                    ts = (1,) if (st == 1 and (typ == 0 or g == 0)) else (0, 1)
                    tsl = slice(ts[0], ts[-1] + 1)
                    for hh in range(2):
                        h = hp * 2 + hh
                        A = psA.next()
                        P.op("pe", [mm(A.ap[:, t, :], wb.ap[:, kc, hh * 128:(hh + 1) * 128], xb[:, kc, t, :],
                                       kc == 0, kc == KC - 1) for t in ts for kc in range(KC)],
                             rd=(wb, B_xb), wr=(A,))
                        if pending is not None:
                            pending()
                            pending = None
                        ob = obp.next()
                        dst = (QT, KT, VT)[typ][g]
                        dst_ap = tt2(dst[h * 128:(h + 1) * 128, st * ST:(st + 1) * ST])[:, tsl, :]
                        key = ("qkv", typ, g, h, st)
                        if typ < 2:
                            qb = qbp.next()
                            P.op("act", I("activation", out=qb.ap[:, tsl, :], in_=A.ap[:, tsl, :], func=AF.Copy),
                                 rd=(A,), wr=(qb,))

                            def cont(A=A, qb=qb, ob=ob, dst_ap=dst_ap, key=key, st=st, ts=ts, tsl=tsl):
                                Bp = psB.next()
                                P.op("pe", [mm(Bp.ap[:, t, :], rmat[:], qb.ap[:, t, :], True, True) for t in ts],
                                     rd=(qb, B_rm), wr=(Bp,))
                                t1 = t1p.next()
                                t2 = t2p.next()
                                cs = slice(2 * st + ts[0], 2 * st + ts[-1] + 1)
                                P.op("dve", I("tensor_tensor", out=t1.ap[:, tsl, :], in0=A.ap[:, tsl, :], in1=cos[:, cs, :],
                                              op=ALU.mult), rd=(A, B_cos), wr=(t1,))
                                P.op("dve", I("tensor_tensor", out=t2.ap[:, tsl, :], in0=Bp.ap[:, tsl, :], in1=sin[:, cs, :],
                                              op=ALU.mult), rd=(Bp, B_sin), wr=(t2,))
                                P.op("pool", I("tensor_tensor", out=ob.ap[:, tsl, :], in0=t1.ap[:, tsl, :],
                                               in1=t2.ap[:, tsl, :], op=ALU.add), rd=(t1, t2), wr=(ob,))
                                P.dma("sp", dst_ap, ob.ap[:, tsl, :], rd=(ob,), wr=(dbuf(key),))
                            pending = cont
                        else:
                            P.op("act", I("activation", out=ob.ap[:, tsl, :], in_=A.ap[:, tsl, :], func=AF.Copy),
                                 rd=(A,), wr=(ob,))
                            P.dma("sp", dst_ap, ob.ap[:, tsl, :], rd=(ob,), wr=(dbuf(key),))
                if pending is not None:
                    pending()
                    pending = None
            P.barrier()
            P.flush()

    if "attn" in phases:
        with contextlib.ExitStack() as es:
            acc = sb(es, "acc", [128, 2, S], F32)
            lds = [[Buf(sb(es, "ld%d_%d" % (i, j), [128, S], BF16)) for j in range(3)] for i in range(2)]
            vbp = RR([sb(es, "vb%d" % i, [128, 32, 128], BF16) for i in range(2)])
            ptp = RR([sb(es, "pt%d" % i, [128, 2, 128], BF16) for i in range(3)])
            rl = sb(es, "rl", [128, S], F32)
            ohp = RR([sb(es, "oh%d" % i, [128, S], BF16) for i in range(2)])
            psS = RR([ps[:, i, 0:256].rearrange("p (a b) -> p a b", b=128) for i in range(3)], excl=True)
            psO = RR([ps[:, 3 + i, 0:256].rearrange("p (a b) -> p a b", b=128) for i in range(3)], excl=True)
            psTp = RR([ps[:, 6 + i, :].rearrange("p (a b) -> p a b", b=128) for i in range(2)], excl=True)
            B_acc, B_rl = Buf(), Buf()
            scale = float(DH ** -0.5)
            NHEADS = test.get("nheads", H)
            li = 0
            items = []
            state = {"first_acc": True, "li": 0}

            def make_group(h, g):
                d = DILS[g]
                nb = 32 // d
                span = 128 * d
                nq0 = HALO0 // span
                nk0 = max(nq0 - 1, 0)
                ctx = {}

                def pre1():
                    lb = lds[state["li"] % 2]
                    state["li"] += 1
                    for j, src in enumerate((QT[g], KT[g], VT[g])):
                        rdb = [dbuf(("qkv", j, g, h, st)) for st in range(NST)] if "qkv" in phases else []
                        rdb = [b_ for b_ in rdb if b_.wr]
                        l0_ = HALO0 if j == 0 else nk0 * span
                        P.dma("sp", lb[j].ap[:, l0_:S], src[h * 128:(h + 1) * 128, l0_:S], rd=rdb, wr=(lb[j],))
                    Vh = lb[2].ap
                    vb = vbp.next()
                    need_bi = [r * nb + n for r in range(d) for n in range(nk0, nb)]
                    first_vb = True
                    for q4 in range(0, len(need_bi), 4):
                        grp = need_bi[q4:q4 + 4]
                        pT_ = psTp.next()
                        inss = []
                        for j, bi in enumerate(grp):
                            r, n = bi // nb, bi % nb
                            s0 = span * n + r
                            inss.append(mm(pT_.ap[:, j, :], Vh[:, s0:s0 + 127 * d + 1:d], identb[:], True, True))
                        P.op("pe", inss, rd=(lb[2], B_id), wr=(pT_,))
                        for j, bi in enumerate(grp):
                            P.op("act", I("activation", out=vb.ap[:, bi, :], in_=pT_.ap[:, j, :], func=AF.Copy),
                                 rd=(pT_,), wr=(vb,), acc=(not first_vb))
                            first_vb = False
                    ctx["lb"], ctx["vb"] = lb, vb

                first = True
                for r in range(d):
                    for n in range(nq0, nb):
                        bi = r * nb + n
                        s0 = span * n + r
                        i_lo = 0
                        if s0 < HALO0:
                            i_lo = -(-(HALO0 - s0) // d)
                        nq = 128 - i_lo
                        if nq <= 0:
                            continue

                        def stage1(bi=bi, s0=s0, i_lo=i_lo, nq=nq, n=n):
                            lb = ctx["lb"]
                            Qh, Kh = lb[0].ap, lb[1].ap
                            qsl = slice(s0 + i_lo * d, s0 + 127 * d + 1, d)
                            cur = slice(s0, s0 + 127 * d + 1, d)
                            prv = slice(s0 - span, s0 - span + 127 * d + 1, d)
                            has_prev = n > 0
                            cur_pad = (s0 + 127 * d) < OWN
                            prv_pad = has_prev and (s0 - span + 127 * d) < OWN
                            assert (s0 >= OWN) or cur_pad
                            Sb = psS.next()
                            bc = biasp if cur_pad else biasb
                            inss = [mm(Sb.ap[:, 1, 0:nq], Kh[:, cur], Qh[:, qsl], True, False),
                                    mm(Sb.ap[:, 1, 0:nq], identb[:], bc[:, 1, i_lo:128], False, True)]
                            if has_prev:
                                bp_ = biasp if prv_pad else biasb
                                inss += [mm(Sb.ap[:, 0, 0:nq], Kh[:, prv], Qh[:, qsl], True, False),
                                         mm(Sb.ap[:, 0, 0:nq], identb[:], bp_[:, 0, i_lo:128], False, True)]
                            P.op("pe", inss, rd=(lb[0], lb[1], B_id, B_mask, B_maskp), wr=(Sb,))
                            pt = ptp.next()
                            lo = 0 if has_prev else 1
                            P.op("act", I("activation", out=pt.ap[:, lo:2, 0:nq], in_=Sb.ap[:, lo:2, 0:nq], func=AF.Exp,
                                          scale=scale), rd=(Sb,), wr=(pt,))
                            return pt

                        def stage2(pt, bi=bi, s0=s0, i_lo=i_lo, nq=nq, n=n):
                            vb = ctx["vb"]
                            qsl = slice(s0 + i_lo * d, s0 + 127 * d + 1, d)
                            has_prev = n > 0
                            Ob = psO.next()
                            inss = []
                            for half in range(2):
                                lc = vb.ap[:, bi, :] if half == 0 else onesb[:]
                                if has_prev:
                                    lp = vb.ap[:, bi - 1, :] if half == 0 else onesb[:]
                                    inss.append(mm(Ob.ap[:, half, 0:nq], lp, pt.ap[:, 0, 0:nq], True, False))
                                inss.append(mm(Ob.ap[:, half, 0:nq], lc, pt.ap[:, 1, 0:nq], not has_prev, True))
                            P.op("pe", inss, rd=(pt, vb, B_ob), wr=(Ob,))
                            P.op("dve", I("tensor_tensor", out=acc[:, :, qsl], in0=Ob.ap[:, :, 0:nq], in1=acc[:, :, qsl],
                                          op=ALU.add), rd=(Ob,), wr=(B_acc,), acc=(not state["first_acc"]))
                            state["first_acc"] = False

                        items.append([pre1 if first else None, stage1, stage2, None])
                        first = False

            def make_fin(h):
                def fin():
                    oh = ohp.next()
                    P.op("dve", I("tensor_scalar", out=rl[:, HALO0:S], in0=acc[:, 1, HALO0:S], scalar1=1e-30, scalar2=None,
                                  op0=ALU.add), rd=(B_acc,), wr=(B_rl,))
                    P.op("dve", I("reciprocal", out=rl[:, HALO0:S], in_=rl[:, HALO0:S]), rd=(), wr=(B_rl,))
                    P.op("dve", I("tensor_tensor", out=oh.ap[:, HALO0:S], in0=acc[:, 0, HALO0:S], in1=rl[:, HALO0:S],
                                  op=ALU.mult), rd=(B_acc, B_rl), wr=(oh,))
                    P.dma("sp", oT[h * 128:(h + 1) * 128, HALO0:S], oh.ap[:, HALO0:S], rd=(oh,), wr=(dbuf(("oT", h)),))
                    P.op("pool", I("memset", ap=acc[:, :, HALO0:S], constant=0.0), wr=(B_acc,))
                    state["first_acc"] = True
                return fin

            for h in range(NHEADS):
                for g in range(3):
                    make_group(h, g)
                    k0 = len(items)
                items[-1][3] = make_fin(h)
            P.op("pool", I("memset", ap=acc[:, :, HALO0:S], constant=0.0), wr=(B_acc,))
            LOOK = 2
            pend = {}
            for i in range(len(items) + LOOK):
                if i < len(items):
                    if items[i][0] is not None:
                        items[i][0]()
                    pend[i] = items[i][1]()
                j = i - LOOK
                if j >= 0:
                    items[j][2](pend.pop(j))
                    if items[j][3] is not None:
                        items[j][3]()
            P.barrier()
            P.flush()

    if "l0" in phases or "l1" in phases:
        with contextlib.ExitStack() as es:
            z = sb(es, "z", [128, KC, 2, 512], F32)
            zb = [[Buf(z[:, c, t, :]) for t in range(2)] for c in range(KC)]
            zc = [(zb[c][0], zb[c][1]) for c in range(KC)]
            xb = sb(es, "xb3", [128, KC, 2, 512], BF16)
            B_xb = Buf()
            hbuf = sb(es, "hbuf", [128, KC, 2, 512], BF16)
            hb = [Buf(hbuf[:, i]) for i in range(KC)]
            w13 = RR([sb(es, "w13_%d" % i, [128, KC, 128], BF16) for i in range(4)])
            w2p = RR([sb(es, "w2_%d" % i, [128, HF, 128], BF16) for i in range(3)])
            sgp = RR([sb(es, "sg%d" % i, [128, 2, 512], F32) for i in range(2)])
            gbp = RR([sb(es, "gb%d" % i, [128, 2, 512], F32) for i in range(1)])
            sqp = RR([sb(es, "sq%d" % i, [128, 512], F32) for i in range(2)])
            mean_t = sb(es, "mean_t", [128, 512], F32)
            var_t = sb(es, "var_t", [128, 512], F32)
            rstd_t = sb(es, "rstd_t", [128, 512], F32)
            nmr_t = sb(es, "nmr_t", [128, 512], F32)
            B_mean, B_var, B_rstd, B_nmr = Buf(), Buf(), Buf(), Buf()
            utail = sb(es, "utail", [128, KC, 16], F32)
            B_ut = [Buf() for _ in range(KC)]
            ubt = [sb(es, "ub%d" % i, [128, 16 + ST], F32) for i in range(2)]
            uat = [sb(es, "ua%d" % i, [128, 16 + ST], F32) for i in range(2)]
            ubp = RR(ubt)
            uap = RR(uat)

            def v8(t):
                return t[0:8, 0:ST].rearrange("p (a b) -> p a b", b=512)

            L8, R8, C8, E8 = v8(ubt[0]), v8(ubt[1]), v8(uat[0]), v8(uat[1])
            G8 = sb(es, "G8", [8, 2, 512], F32)
            B_L8, B_R8, B_C8, B_E8, B_G8 = Buf(), Buf(), Buf(), Buf(), Buf()
            ps2 = RR([ps[:, 2 * i:2 * i + 2, :] for i in range(4)], excl=True)

            geo = {"nt": 2, "tw": 512}

            def W(ap):
                return ap[:, 0:geo["nt"], 0:geo["tw"]]

            def Wt(ap, t):
                return ap[:, t, 0:geo["tw"]]

            def Wf(ap):
                return ap[:, 0:geo["tw"]]

            def cols(ap2d):
                return ap2d.rearrange("p (a b) -> p a b", b=geo["tw"])

            def cols3(ap3d):
                return ap3d.rearrange("p c (a b) -> p c a b", b=geo["tw"])

            def mm2(psb, lhs_list, rhs_of, rd):
                n = len(lhs_list)
                P.op("pe", [mm(Wt(psb.ap, t), lhs_list[k], rhs_of(k, t), k == 0, k == n - 1)
                            for t in range(geo["nt"]) for k in range(n)], rd=rd, wr=(psb,))

            def xb_rhs(k, t):
                return Wt(xb[:, k], t)

            def load_xb_from_z():
                for c in range(KC):
                    if c % 2 == 0:
                        P.op("act", I("activation", out=W(xb[:, c]), in_=W(z[:, c]), func=AF.Copy), rd=zc[c], wr=(B_xb,), acc=True)
                    else:
                        P.op("pool", I("tensor_copy", out=W(xb[:, c]), in_=W(z[:, c])), rd=zc[c], wr=(B_xb,), acc=True)

            def layer_norm(vg, vb_):
                for t in range(geo["nt"]):
                    pss = ps2.next()
                    P.op("pe", [mm(Wt(pss.ap, 0), ones32[:], Wt(z[:, c], t), c == 0, c == KC - 1) for c in range(KC)],
                         rd=[zb[c][t] for c in range(KC)] + [B_o32], wr=(pss,))
                    for c in range(KC):
                        sq = sqp.next()
                        P.op("act", I("activation", out=Wf(sq.ap), in_=Wt(z[:, c], t), func=AF.Square), rd=(zb[c][t],), wr=(sq,))
                        P.op("pe", mm(Wt(pss.ap, 1), ones32[:], Wf(sq.ap), c == 0, c == KC - 1), rd=(sq,), wr=(pss,), acc=True)
                    P.op("dve", I("tensor_scalar", out=Wf(mean_t), in0=Wt(pss.ap, 0), scalar1=1.0 / D, scalar2=None,
                                  op0=ALU.mult), rd=(pss,), wr=(B_mean,))
                    P.op("dve", I("tensor_tensor", out=Wf(var_t), in0=Wf(mean_t), in1=Wf(mean_t), op=ALU.mult),
                         rd=(B_mean,), wr=(B_var,))
                    P.op("dve", I("scalar_tensor_tensor", out=Wf(var_t), in0=Wt(pss.ap, 1), scalar=1.0 / D, in1=Wf(var_t),
                                  op0=ALU.mult, op1=ALU.subtract), rd=(pss,), wr=(B_var,))
                    P.op("dve", I("tensor_scalar", out=Wf(var_t), in0=Wf(var_t), scalar1=EPS, scalar2=None,
                                  op0=ALU.add), rd=(), wr=(B_var,))
                    P.op("act", I("activation", out=Wf(rstd_t), in_=Wf(var_t), func=AF.Sqrt), rd=(B_var,), wr=(B_rstd,))
                    P.op("dve", I("reciprocal", out=Wf(rstd_t), in_=Wf(rstd_t)), rd=(), wr=(B_rstd,))
                    P.op("dve", I("scalar_tensor_tensor", out=Wf(nmr_t), in0=Wf(mean_t), scalar=-1.0, in1=Wf(rstd_t),
                                  op0=ALU.mult, op1=ALU.mult), rd=(B_mean, B_rstd), wr=(B_nmr,))
                    for c in range(KC):
                        zz = Wt(z[:, c], t)
                        P.op("dve", I("tensor_tensor", out=zz, in0=zz, in1=Wf(rstd_t), op=ALU.mult),
                             rd=(B_rstd,), wr=(zb[c][t],))
                        P.op("pool", I("tensor_tensor", out=zz, in0=zz, in1=Wf(nmr_t), op=ALU.add),
                             rd=(B_nmr,), wr=(zb[c][t],))
                        P.op("act", I("activation", out=zz, in_=zz, func=AF.Identity,
                                      scale=vecs[:, vg, c:c + 1], bias=vecs[:, vb_, c:c + 1]),
                             rd=(B_vecs,), wr=(zb[c][t],))

            def gemm_resid(wdram, src_bufs, rhs_of, nk):
                reqs = [(wdram[:, :, oc * 128:(oc + 1) * 128], (lambda ap, nk=nk: ap[:, :nk, :])) for oc in range(KC)]
                stream = Stream(P, w13, reqs, depth=2)
                for oc in range(KC):
                    wb = stream.get(oc)
                    psb = ps2.next()
                    mm2(psb, [wb.ap[:, k, :] for k in range(nk)], rhs_of, rd=[wb] + list(src_bufs))
                    P.op("dve", I("scalar_tensor_tensor", out=W(z[:, oc]), in0=W(z[:, oc]), scalar=ALPHA, in1=W(psb.ap),
                                  op0=ALU.mult, op1=ALU.add), rd=(psb,), wr=zc[oc])

            def ffn(experts):
                first = True
                plan = []
                for (w1v, w3v, w2v, ge) in experts:
                    for part in range(NSPLIT):
                        reqs = []
                        for fi in range(HF):
                            f = part * HF + fi
                            reqs.append((w1v[:, :, f * 128:(f + 1) * 128], (lambda ap: ap[:])))
                            reqs.append((w3v[:, :, f * 128:(f + 1) * 128], (lambda ap: ap[:])))
                        s13 = Stream(P, w13, reqs, depth=2)
                        reqs2 = [(w2v[:, part * HF:(part + 1) * HF, oc * 128:(oc + 1) * 128], (lambda ap: ap[:]))
                                 for oc in range(KC)]
                        s2 = Stream(P, w2p, reqs2, depth=2)
                        plan.append((ge, part, s13, s2))
                plan[0][2].prefetch(4)
                gb = None
                for pi, (ge, part, stream, stream2) in enumerate(plan):
                    if ge is not None and part == 0:
                        gb = gbp.next()
                        psb = ps2.next()
                        P.op("pe", [mm(Wt(psb.ap, t), selb[:, ge, :], Wt(G8, t), True, True) for t in range(geo["nt"])],
                             rd=(B_G8, B_selb), wr=(psb,))
                        P.op("act", I("activation", out=W(gb.ap), in_=W(psb.ap), func=AF.Copy), rd=(psb,), wr=(gb,))
                    stream2.prefetch(3)
                    for fi in range(HF):
                        wb1 = stream.get(2 * fi)
                        wb3 = stream.get(2 * fi + 1)
                        pg = ps2.next()
                        pu = ps2.next()
                        mm2(pg, [wb1.ap[:, k, :] for k in range(KC)], xb_rhs, rd=(wb1, B_xb))
                        mm2(pu, [wb3.ap[:, k, :] for k in range(KC)], xb_rhs, rd=(wb3, B_xb))
                        sg = sgp.next()
                        P.op("act", I("activation", out=W(sg.ap), in_=W(pg.ap), func=AF.Silu), rd=(pg,), wr=(sg,))
                        if ge is not None:
                            P.op("dve", I("tensor_tensor", out=W(sg.ap), in0=W(pu.ap), in1=W(sg.ap), op=ALU.mult),
                                 rd=(pu,), wr=(sg,))
                            P.op("dve", I("tensor_tensor", out=W(hbuf[:, fi]), in0=W(sg.ap), in1=W(gb.ap), op=ALU.mult),
                                 rd=(sg, gb), wr=(hb[fi],))
                        else:
                            P.op("dve", I("tensor_tensor", out=W(hbuf[:, fi]), in0=W(pu.ap), in1=W(sg.ap), op=ALU.mult),
                                 rd=(pu, sg), wr=(hb[fi],))
                    if pi + 1 < len(plan):
                        plan[pi + 1][2].prefetch(4)
                    for oc in range(KC):
                        wb = stream2.get(oc)
                        py = ps2.next()
                        nt_ = geo["nt"]
                        if oc == 0:
                            P.op("pe", [mm(Wt(py.ap, t), wb.ap[:, k, :], Wt(hbuf[:, k], t), k == 0, False)
                                        for t in range(nt_) for k in range(HF - 1)], rd=[wb] + hb[:HF - 1], wr=(py,))
                            P.op("pe", [mm(Wt(py.ap, t), wb.ap[:, HF - 1, :], Wt(hbuf[:, HF - 1], t), False, True)
                                        for t in range(nt_)], rd=[wb, hb[HF - 1]], wr=(py,), acc=True)
                        else:
                            mm2(py, [wb.ap[:, k, :] for k in range(HF)], (lambda k, t: Wt(hbuf[:, k], t)),
                                rd=[wb] + hb[:HF])
                        if first:
                            P.op("dve", I("scalar_tensor_tensor", out=W(z[:, oc]), in0=W(z[:, oc]), scalar=ALPHA,
                                          in1=W(py.ap), op0=ALU.mult, op1=ALU.add), rd=(py,), wr=zc[oc])
                        else:
                            P.op("dve", I("tensor_tensor", out=W(z[:, oc]), in0=W(py.ap), in1=W(z[:, oc]), op=ALU.add),
                                 rd=(py,), wr=zc[oc])
                    first = False

            def ple(layer, p0, vbias, out_ap_of, out_key):
                ntok = geo["nt"] * geo["tw"]
                load_xb_from_z()
                pb = hbuf[:, 0:2]
                P.dma("pool", pb[:, :, 0:geo["nt"], 0:geo["tw"]], cols3(chunkview(pT[layer])[:, :, p0:p0 + ntok]),
                      wr=(hb[0], hb[1]))
                wgv = chunkview(ple_w_gate[layer])
                wpv = chunkview(ple_w_proj[layer])
                reqs = []
                for oc in range(KC):
                    reqs.append((wgv[:, :, oc * 128:(oc + 1) * 128], (lambda ap: ap[:])))
                    reqs.append((wpv[:, :, oc * 128:(oc + 1) * 128], (lambda ap: ap[:, 0:2, :])))
                stream = Stream(P, w13, reqs, depth=2)
                for oc in range(KC):
                    wg = stream.get(2 * oc)
                    wp = stream.get(2 * oc + 1)
                    pg = ps2.next()
                    pp = ps2.next()
                    mm2(pg, [wg.ap[:, k, :] for k in range(KC)], xb_rhs, rd=(wg, B_xb))
                    mm2(pp, [wp.ap[:, k, :] for k in range(2)], (lambda k, t: Wt(pb[:, k], t)), rd=(wp, hb[0], hb[1]))
                    sg = sgp.next()
                    P.op("act", I("activation", out=W(sg.ap), in_=W(pg.ap), func=AF.Sigmoid,
                                  bias=vecs[:, vbias, oc:oc + 1], scale=1.0), rd=(pg, B_vecs), wr=(sg,))
                    P.op("dve", I("tensor_tensor", out=W(sg.ap), in0=W(pp.ap), in1=W(sg.ap), op=ALU.mult), rd=(pp,), wr=(sg,))
                    P.op("pool", I("tensor_tensor", out=W(z[:, oc]), in0=W(z[:, oc]), in1=W(sg.ap), op=ALU.add),
                         rd=(sg,), wr=zc[oc])
                    if out_ap_of is not None:
                        P.dma("sp", cols(out_ap_of(oc)), W(z[:, oc]), rd=zc[oc], wr=(dbuf((out_key, oc, p0)),))

            def dump(name, e0):
                if name in dbg:
                    ntok = geo["nt"] * geo["tw"]
                    for oc in range(KC):
                        P.dma("sp", cols(dbg[name][oc * 128:(oc + 1) * 128, e0:e0 + ntok]), W(z[:, oc]),
                              rd=zc[oc], wr=(dbuf((name, oc, e0)),))

            P.op("pool", I("memset", ap=utail[:], constant=0.0), wr=B_ut)
            TILES = test.get("tiles", [(HALO0, 1, 128), (OWN, 2, 512), (OWN + ST, 2, 512)])
            for (e0, nt_, tw_) in TILES:
                geo["nt"], geo["tw"] = nt_, tw_
                ntok = nt_ * tw_
                is_halo = e0 < OWN
                if "l0" in phases:
                    for c in range(KC):
                        P.dma("sp", W(z[:, c]), cols(xT[c * 128:(c + 1) * 128, e0:e0 + ntok]), wr=zc[c])
                    P.dma("sp", xb[:, :, 0:nt_, 0:tw_], cols3(chunkview(oT.ap())[:, :, e0:e0 + ntok]),
                          rd=[dbuf(("oT", h)) for h in range(H)] if "attn" in phases else (), wr=(B_xb,))
                    gemm_resid(chunkview(w_o.ap()), (B_xb,), xb_rhs, KC)
                    layer_norm(V_LN_MIX_G0, V_LN_MIX_B0)
                    dump("x1", e0)
                    load_xb_from_z()
                    ffn([(chunkview(ffn_w1.ap()), chunkview(ffn_w3.ap()), chunkview(ffn_w2.ap()), None)])
                    layer_norm(V_LN_FFN_G0, V_LN_FFN_B0)
                    dump("x2", e0)
                    ple(0, e0 - HALO0, V_PLE_B0, (lambda oc, e0=e0, ntok=ntok: x3T[oc * 128:(oc + 1) * 128, e0:e0 + ntok]), "x3")
                if "l1" in phases:
                    if "l0" not in phases:
                        for c in range(KC):
                            P.dma("sp", W(z[:, c]), cols(x3T[c * 128:(c + 1) * 128, e0:e0 + ntok]), wr=zc[c])
                    load_xb_from_z()
                    winv = chunkview(pool_w_in.ap())
                    stream = Stream(P, w13, [(winv[:, :, oc * 128:(oc + 1) * 128], (lambda ap: ap[:])) for oc in range(KC)],
                                    depth=2)
                    for oc in range(KC):
                        wb = stream.get(oc)
                        pu = ps2.next()
                        mm2(pu, [wb.ap[:, k, :] for k in range(KC)], xb_rhs, rd=(wb, B_xb))
                        ub = ubp.next()
                        gi = oc // 4
                        if is_halo:
                            P.op("act", I("activation", out=cols(ub.ap[:, 16:16 + ntok]), in_=W(pu.ap), func=AF.Copy),
                                 rd=(pu,), wr=(ub,))
                            P.op("pool", I("tensor_tensor", out=utail[:, oc, :], in0=ub.ap[:, ntok:ntok + 16], in1=hflag[:],
                                           op=ALU.mult), rd=(ub, B_hflag), wr=(B_ut[oc],))
                            continue
                        ua = uap.next()
                        P.op("pool", I("tensor_copy", out=ub.ap[:, 0:16], in_=utail[:, oc, :]), rd=(B_ut[oc],), wr=(ub,))
                        P.op("act", I("activation", out=cols(ub.ap[:, 16:16 + ntok]), in_=W(pu.ap), func=AF.Copy),
                             rd=(pu,), wr=(ub,), acc=True)
                        P.op("pool", I("tensor_copy", out=utail[:, oc, :], in_=ub.ap[:, ntok:ntok + 16]), rd=(ub,), wr=(B_ut[oc],))
                        P.op("dve", I("tensor_tensor", out=ua.ap[:, 1:16 + ntok], in0=ub.ap[:, 1:16 + ntok],
                                      in1=ub.ap[:, 0:15 + ntok], op=ALU.add), rd=(ub,), wr=(ua,))
                        sh, lo = 2, 1
                        for _ in range(gi):
                            lo2 = lo + sh
                            ua2 = uap.next()
                            P.op("dve", I("tensor_tensor", out=ua2.ap[:, lo2:16 + ntok], in0=ua.ap[:, lo2:16 + ntok],
                                          in1=ua.ap[:, lo2 - sh:16 + ntok - sh], op=ALU.add), rd=(ua,), wr=(ua2,))
                            ua, lo, sh = ua2, lo2, sh * 2
                        w = POOLW[gi]
                        if e0 == OWN:
                            P.op("dve", I("tensor_tensor", out=ua.ap[:, 16:32], in0=ua.ap[:, 16:32], in1=corr[:, gi, :],
                                          op=ALU.mult), rd=(B_corr,), wr=(ua,))
                        P.op("dve", I("scalar_tensor_tensor", out=W(hbuf[:, oc]), in0=cols(ua.ap[:, 16:16 + ntok]), scalar=1.0 / w,
                                      in1=cols(ub.ap[:, 16:16 + ntok]), op0=ALU.mult, op1=ALU.subtract),
                             rd=(ua, ub), wr=(hb[oc],))
                    if is_halo:
                        continue
                    reqs = []
                    for oc in range(KC):
                        gi, ol = oc // 4, oc % 4
                        reqs.append((chunkview(pool_w_group[gi])[:, :, ol * 128:(ol + 1) * 128], (lambda ap: ap[:, 0:4, :])))
                    stream = Stream(P, w13, reqs, depth=2)
                    for oc in range(KC):
                        gi = oc // 4
                        wb = stream.get(oc)
                        py = ps2.next()
                        mm2(py, [wb.ap[:, k, :] for k in range(4)], (lambda k, t, gi=gi: Wt(hbuf[:, gi * 4 + k], t)),
                            rd=[wb] + hb[gi * 4:gi * 4 + 4])
                        P.op("act", I("activation", out=W(xb[:, oc]), in_=W(py.ap), func=AF.Identity,
                                      scale=vecs[:, V_POOL_SCALE, oc:oc + 1], bias=0.0),
                             rd=(py, B_vecs), wr=(B_xb,), acc=(oc > 0))
                    gemm_resid(chunkview(pool_w_o.ap()), (B_xb,), xb_rhs, KC)
                    layer_norm(V_LN_MIX_G1, V_LN_MIX_B1)
                    dump("x4", e0)
                    pl = ps2.next()
                    P.op("pe", [mm(Wt(pl.ap[0:8], t), wr32[:, c, :], Wt(z[:, c], t), c == 0, c == KC - 1)
                                for t in range(nt_) for c in range(KC)],
                         rd=[zb[c][t] for c in range(KC) for t in range(2)] + [B_wr], wr=(pl,))
                    P.op("act", I("activation", out=W(L8), in_=W(pl.ap[0:8]), func=AF.Copy), rd=(pl,), wr=(B_L8,))
                    P.op("act", I("activation", out=W(E8), in_=W(L8), func=AF.Exp), rd=(B_L8,), wr=(B_E8,))
                    P.op("dve", I("memset", ap=W(R8), constant=0.0), wr=(B_R8,))
                    for e2 in range(NE):
                        pb_ = ps2.next()
                        P.op("pe", [mm(Wt(pb_.ap[0:8], t), selb[:, e2, 0:8], Wt(L8, t), True, True) for t in range(nt_)],
                             rd=(B_L8, B_selb), wr=(pb_,))
                        P.op("dve", I("tensor_tensor", out=W(C8), in0=W(pb_.ap[0:8]), in1=W(L8), op=ALU.is_gt),
                             rd=(pb_, B_L8), wr=(B_C8,))
                        P.op("dve", I("tensor_tensor", out=W(R8), in0=W(R8), in1=W(C8), op=ALU.add), rd=(B_C8,), wr=(B_R8,))
                    P.op("dve", I("scalar_tensor_tensor", out=W(E8), in0=W(R8), scalar=1.5, in1=W(E8), op0=ALU.is_lt,
                                  op1=ALU.mult), rd=(B_R8,), wr=(B_E8,))
                    pd = ps2.next()
                    P.op("pe", [mm(Wt(pd.ap[0:8], t), ones32[0:8, 0:8], Wt(E8, t), True, True) for t in range(nt_)],
                         rd=(B_E8, B_o32), wr=(pd,))
                    P.op("dve", I("reciprocal", out=W(C8), in_=W(pd.ap[0:8])), rd=(pd,), wr=(B_C8,))
                    P.op("dve", I("tensor_tensor", out=W(G8), in0=W(E8), in1=W(C8), op=ALU.mult), rd=(B_E8, B_C8), wr=(B_G8,))
                    load_xb_from_z()
                    ffn([(chunkview(moe_w1[e_]), chunkview(moe_w3[e_]), chunkview(moe_w2[e_]), e_) for e_ in range(ne_run)])
                    layer_norm(V_LN_FFN_G1, V_LN_FFN_B1)
                    dump("x5", e0)
                    ple(1, e0 - HALO0, V_PLE_B1,
                        (lambda oc, e0=e0, ntok=ntok: outT[oc * 128:(oc + 1) * 128, e0 - OWN:e0 - OWN + ntok]), "out")
            P.barrier()
            P.flush()
    es0.close()
    P.stack.close()
    build.last_ninstr = P.ninstr
    return nc


def host_consts(half):
    hd = DH // 2
    inv = (10000.0 ** (-np.arange(hd, dtype=np.float32) / hd)).astype(np.float32)
    pos = (np.arange(S) - (0 if half == 1 else OWN)).astype(np.float32)
    ang = pos[:, None] * inv[None, :]
    c = np.cos(ang).astype(np.float32).T
    s = np.sin(ang).astype(np.float32).T
    cosT = np.concatenate([c, c], 0)
    sinT = np.concatenate([-s, s], 0)
    rm = np.zeros((128, 128), np.float32)
    for m in range(128):
        rm[(m + 64) % 128, m] = 1.0
    i = np.arange(128)
    mask = np.zeros((128, 2, 128), np.float32)
    mask[:, 0, :] = (i[:, None] >= i[None, :])
    mask[:, 1, :] = (i[:, None] <= i[None, :])
    NEG = -30000.0
    mask = np.where(mask > 0, 0.0, NEG).astype(np.float32)
    maskp = mask.copy() if half == 1 else np.full_like(mask, NEG)
    hflag = np.full((128, 16), 1.0 if half == 1 else 0.0, np.float32)
    ident = np.eye(128, dtype=np.float32)
    selb = np.zeros((8, NE, 128), np.float32)
    for e in range(NE):
        selb[e, e, :] = 1.0
    corr = np.ones((128, 4, 16), np.float32)
    if half == 0:
        for gi, w in enumerate(POOLW):
            t = np.arange(16)
            corr[:, gi, :] = (w / np.minimum(t + 1, w)).astype(np.float32)[None, :]
    return dict(cosT=np.ascontiguousarray(cosT), sinT=np.ascontiguousarray(sinT), rmat=rm, maskT=mask, maskP=maskp,
                hflag=hflag, ident=ident, selb=selb, corr=corr)


def pack_vecs(inp):
    def lay(v):
        return np.asarray(v, np.float32).reshape(KC, 128).T
    vs = [inp["ln_mix_g"][0], inp["ln_mix_b"][0], inp["ln_ffn_g"][0], inp["ln_ffn_b"][0],
          inp["ln_mix_g"][1], inp["ln_mix_b"][1], inp["ln_ffn_g"][1], inp["ln_ffn_b"][1],
          inp["ple_b_gate"][0], inp["ple_b_gate"][1], np.asarray(inp["pool_scale"][0]).reshape(-1)]
    return np.ascontiguousarray(np.stack([lay(v) for v in vs], axis=1))


def shared_inputs(inp):
    f = lambda a: np.ascontiguousarray(np.asarray(a, np.float32))
    m = dict(
        attn_w_qkv=f(inp["attn_w_qkv"][0]), attn_w_o=f(inp["attn_w_o"][0]),
        pool_w_in=f(inp["pool_w_in"][0]), pool_w_group=f(inp["pool_w_group"][0]), pool_w_o=f(inp["pool_w_o"][0]),
        ffn_w1=f(inp["ffn_w1"][0]), ffn_w3=f(inp["ffn_w3"][0]), ffn_w2=f(inp["ffn_w2"][0]),
        moe_router=f(inp["moe_router"][0]), moe_w1=f(inp["moe_w1"][0]), moe_w3=f(inp["moe_w3"][0]),
        moe_w2=f(inp["moe_w2"][0]), ple_w_proj=f(inp["ple_w_proj"]), ple_w_gate=f(inp["ple_w_gate"]),
        vecs=pack_vecs(inp),
    )
    return m


def core_inputs(x, p, b, half):
    m = host_consts(half)
    xT = np.zeros((D, S), np.float32)
    pT = np.zeros((2, 256, NQ), np.float32)
    if half == 1:
        xT[:] = x[b].T
        pT[:] = np.transpose(p[:, b, HALO0:S], (0, 2, 1))
    else:
        xT[:, OWN:] = x[b, 0:OWN].T
        pT[:, :, OWN - HALO0:] = np.transpose(p[:, b, 0:OWN], (0, 2, 1))
    m["xT"] = xT
    m["pT"] = pT
    return m


def kernel(**inp):
    ncores = 8
    x = np.asarray(inp["x"], np.float32)
    p = np.asarray(inp["p"], np.float32)
    shared = shared_inputs(inp)
    in_maps = []
    for c in range(ncores):
        m = dict(shared)
        m.update(core_inputs(x, p, c // 2, c % 2))
        in_maps.append(m)
    nc = build(ncores)
    res = run_bass_kernel_spmd(nc, in_maps, core_ids=list(range(ncores)))
    out = np.zeros((4, S, D), np.float32)
    for c in range(ncores):
        b, half = c // 2, c % 2
        out[b, half * OWN:(half + 1) * OWN, :] = res.results[c]["outT"].T
    return out
```
